# Optimizing a Trainium2 kernel written in Bass

```python
import math
import jax
import jax.numpy as jnp
from jax import lax
import numpy as np


D_MODEL = 1024
BATCH = 8
SEQ = 8192
DEPTH = 4

D_MIX = D_MODEL
HEAD_DIM = 64
ATTN_WIDTH = D_MIX // 2
ATTN_HEADS = ATTN_WIDTH // HEAD_DIM
ATTN_KV_HEADS = 2
ATTN_REP = ATTN_HEADS // ATTN_KV_HEADS
ATTN_SCALE = HEAD_DIM ** -0.5
ROPE_DIM = HEAD_DIM // 4
ROPE_THETA = 500000.0
IDX_HEADS = 8
IDX_DIM = HEAD_DIM
IDX_W_SCALE = (IDX_HEADS ** -0.5) * (IDX_DIM ** -0.5)
TOPK_MAX = 256
Q_BLOCK = 128
S5_WIDTH = D_MIX // 4
S5_GROUP_CH = 16
S5_GROUPS = S5_WIDTH // S5_GROUP_CH
S5_STATE = 64
SSD_WIDTH = D_MIX - ATTN_WIDTH - S5_WIDTH
SSD_HEAD_DIM = 64
SSD_HEADS = SSD_WIDTH // SSD_HEAD_DIM
SSD_NGROUPS = 2
SSD_STATE = 64
SSD_CONV = 4
SSD_CHUNK = 128
SSD_XBC = SSD_WIDTH + 2 * SSD_NGROUPS * SSD_STATE
D_FF = 4 * D_MODEL
EPS = 1e-6
IN_SIZES = (ATTN_WIDTH, ATTN_KV_HEADS * HEAD_DIM, ATTN_KV_HEADS * HEAD_DIM,
            IDX_HEADS * IDX_DIM, IDX_DIM, IDX_HEADS,
            S5_WIDTH,
            SSD_WIDTH, SSD_XBC, SSD_HEADS)
N_IN = sum(IN_SIZES)

kernel_name = 'hybrid_dsa_s5_ssd_block'


def _rms(x):
    xf = x.astype(jnp.float32)
    return xf * lax.rsqrt(jnp.mean(xf * xf, axis=-1, keepdims=True) + EPS)


def _rmsnorm(x, g):
    return (_rms(x) * g).astype(x.dtype)


def _split(z, sizes):
    out = []
    start = 0
    for n in sizes:
        out.append(z[..., start:start + n])
        start += n
    return out


def _rope_tables(s):
    pos = jnp.arange(s, dtype=jnp.float32)
    inv_freq = ROPE_THETA ** (-jnp.arange(0, ROPE_DIM, 2, dtype=jnp.float32) / ROPE_DIM)
    ang = pos[:, None] * inv_freq[None, :]
    return jnp.cos(ang), jnp.sin(ang)


def _partial_rope(x, cos, sin):
    half = ROPE_DIM // 2
    x1 = x[..., :half]
    x2 = x[..., half:ROPE_DIM]
    rot = jnp.concatenate([x1 * cos - x2 * sin, x2 * cos + x1 * sin,
                           x[..., ROPE_DIM:].astype(jnp.float32)], axis=-1)
    return rot.astype(x.dtype)


def _gather_rows(table, idx):
    return jax.vmap(lambda t, i: t[i])(table, idx)


def _dsa_mixer(q, k, v, qi, ki, wi, q_g, k_g, ki_g, cos, sin):
    b, s, _ = q.shape
    cos_h, sin_h = cos[:, None, :], sin[:, None, :]
    q = _partial_rope(_rmsnorm(q.reshape(b, s, ATTN_HEADS, HEAD_DIM), q_g), cos_h, sin_h)
    k = _partial_rope(_rmsnorm(k.reshape(b, s, ATTN_KV_HEADS, HEAD_DIM), k_g), cos_h, sin_h)
    v = v.reshape(b, s, ATTN_KV_HEADS, HEAD_DIM)
    qi = _partial_rope(qi.reshape(b, s, IDX_HEADS, IDX_DIM), cos_h, sin_h)
    ki = _partial_rope(_rmsnorm(ki, ki_g), cos, sin)
    wi = wi.astype(jnp.float32) * IDX_W_SCALE
    topk = min(TOPK_MAX, s // 4)
    nb = s // Q_BLOCK
    key_pos = jnp.arange(s, dtype=jnp.int32)

    def blocks(a):
        return a.reshape((b, nb, Q_BLOCK) + a.shape[2:]).swapaxes(0, 1)

    def attend(args):
        q_b, qi_b, wi_b, start = args
        q_pos = start + jnp.arange(Q_BLOCK, dtype=jnp.int32)
        idx_logits = jnp.einsum('bqhd,bsd->bqhs', qi_b, ki).astype(jnp.float32)
        score = jnp.einsum('bqh,bqhs->bqs', wi_b, jax.nn.relu(idx_logits))
        admissible = key_pos[None, :] <= q_pos[:, None]
        score = jnp.where(admissible[None], score, -jnp.inf)
        _, sel = lax.top_k(score, topk)
        valid = sel <= q_pos[None, :, None]
        k_sel = _gather_rows(k, sel)
        v_sel = _gather_rows(v, sel)
        qg = q_b.reshape(b, Q_BLOCK, ATTN_KV_HEADS, ATTN_REP, HEAD_DIM)
        logits = jnp.einsum('bqgrd,bqkgd->bqgrk', qg, k_sel).astype(jnp.float32) * ATTN_SCALE
        logits = jnp.where(valid[:, :, None, None, :], logits, -jnp.inf)
        p = jax.nn.softmax(logits, axis=-1).astype(v_sel.dtype)
        o = jnp.einsum('bqgrk,bqkgd->bqgrd', p, v_sel)
        return o.reshape(b, Q_BLOCK, ATTN_WIDTH)

    starts = jnp.arange(nb, dtype=jnp.int32) * Q_BLOCK
    out = lax.map(attend, (blocks(q), blocks(qi), blocks(wi), starts))
    return out.swapaxes(0, 1).reshape(b, s, ATTN_WIDTH)


def _complex_affine_combine(e1, e2):
    a1r, a1i, b1r, b1i = e1
    a2r, a2i, b2r, b2i = e2
    ar = a2r * a1r - a2i * a1i
    ai = a2r * a1i + a2i * a1r
    br = a2r * b1r - a2i * b1i + b2r
    bi = a2r * b1i + a2i * b1r + b2i
    return (ar, ai, br, bi)


def _s5_mixer(u, lam_re, lam_im, log_step, b_re, b_im, c_re, c_im, d_skip, glu_w, glu_b):
    b, s, _ = u.shape
    uf = u.astype(jnp.float32).reshape(b, s, S5_GROUPS, S5_GROUP_CH)
    step = jnp.exp(log_step.astype(jnp.float32))[:, None]
    lr = lam_re.astype(jnp.float32)
    li = lam_im.astype(jnp.float32)
    mag = jnp.exp(lr * step)
    ab_re = mag * jnp.cos(li * step)
    ab_im = mag * jnp.sin(li * step)
    den = lr * lr + li * li
    cr = ((ab_re - 1.0) * lr + ab_im * li) / den
    ci = (ab_im * lr - (ab_re - 1.0) * li) / den
    bb_re = cr[..., None] * b_re - ci[..., None] * b_im
    bb_im = cr[..., None] * b_im + ci[..., None] * b_re
    bu_re = jnp.einsum('bsgh,gph->bsgp', uf, bb_re)
    bu_im = jnp.einsum('bsgh,gph->bsgp', uf, bb_im)
    a_re = jnp.broadcast_to(ab_re, bu_re.shape)
    a_im = jnp.broadcast_to(ab_im, bu_re.shape)
    _, _, xr, xi = lax.associative_scan(_complex_affine_combine, (a_re, a_im, bu_re, bu_im), axis=1)
    y = jnp.einsum('bsgp,ghp->bsgh', xr, c_re) - jnp.einsum('bsgp,ghp->bsgh', xi, c_im)
    y = y.reshape(b, s, S5_WIDTH) + d_skip * u.astype(jnp.float32)
    y = jax.nn.gelu(y)
    return (y * jax.nn.sigmoid(y @ glu_w + glu_b)).astype(u.dtype)


def _causal_dwconv(x, w, bias):
    out = lax.conv_general_dilated(x, w[:, None, :].astype(x.dtype), window_strides=(1,),
                                   padding=((SSD_CONV - 1, 0),),
                                   dimension_numbers=('NWC', 'WIO', 'NWC'),
                                   feature_group_count=x.shape[-1])
    return out + bias


def _ssd_chunked(x, a_dt, bmat, cmat):
    b, s, h, p = x.shape
    n = bmat.shape[-1]
    c, l = s // SSD_CHUNK, SSD_CHUNK
    x = x.reshape(b, c, l, h, p)
    bmat = bmat.reshape(b, c, l, h, n)
    cmat = cmat.reshape(b, c, l, h, n)
    a = a_dt.reshape(b, c, l, h).transpose(0, 3, 1, 2)
    a_cs = jnp.cumsum(a, axis=-1)
    seg = a_cs[..., :, None] - a_cs[..., None, :]
    causal = jnp.tril(jnp.ones((l, l), dtype=bool))
    decay = jnp.exp(jnp.where(causal, seg, -jnp.inf))
    scores = jnp.einsum('bclhn,bcshn->bhcls', cmat, bmat) * decay
    y_diag = jnp.einsum('bhcls,bcshp->bclhp', scores, x)
    decay_states = jnp.exp(a_cs[..., -1:] - a_cs)
    states = jnp.einsum('bclhn,bhcl,bclhp->bchpn', bmat, decay_states, x)
    chunk_decay = jnp.exp(a_cs[..., -1])

    def step(carry, inp):
        st, dec = inp
        return carry * dec[:, :, None, None] + st, carry

    init = jnp.zeros((b, h, p, n), dtype=x.dtype)
    _, prev = lax.scan(step, init, (states.transpose(1, 0, 2, 3, 4), chunk_decay.transpose(2, 0, 1)))
    prev = prev.transpose(1, 0, 2, 3, 4)
    y_off = jnp.einsum('bclhn,bchpn,bhcl->bclhp', cmat, prev, jnp.exp(a_cs))
    return (y_diag + y_off).reshape(b, s, h, p)


def _ssd_mixer(z, xbc, dt_raw, conv_w, conv_b, dt_bias, a_log, d_skip, norm_g):
    b, s, _ = xbc.shape
    xbc = jax.nn.silu(_causal_dwconv(xbc, conv_w, conv_b))
    xs, bm, cm = _split(xbc, (SSD_WIDTH, SSD_NGROUPS * SSD_STATE, SSD_NGROUPS * SSD_STATE))
    xs = xs.reshape(b, s, SSD_HEADS, SSD_HEAD_DIM).astype(jnp.float32)
    rep = SSD_HEADS // SSD_NGROUPS
    bm = jnp.repeat(bm.reshape(b, s, SSD_NGROUPS, SSD_STATE).astype(jnp.float32), rep, axis=2)
    cm = jnp.repeat(cm.reshape(b, s, SSD_NGROUPS, SSD_STATE).astype(jnp.float32), rep, axis=2)
    dt = jax.nn.softplus((dt_raw + dt_bias).astype(jnp.float32))
    a = -jnp.exp(a_log.astype(jnp.float32))
    y = _ssd_chunked(xs * dt[..., None], dt * a, bm, cm)
    y = y + d_skip[:, None] * xs
    y = y.reshape(b, s, SSD_WIDTH) * jax.nn.silu(z.astype(jnp.float32))
    y = _rms(y.reshape(b, s, SSD_NGROUPS, SSD_WIDTH // SSD_NGROUPS)).reshape(b, s, SSD_WIDTH)
    return (y * norm_g).astype(z.dtype)


def setup_inputs(seed: int = 0) -> dict:
    key = jax.random.key(seed)
    ks = jax.random.split(key, 26)
    f32 = jnp.float32

    def nrm(k, shape, scale):
        return jax.random.normal(k, shape, f32) * scale

    def gain(k, shape):
        return 1.0 + 0.02 * jax.random.normal(k, shape, f32)

    lam_im = jnp.broadcast_to(jnp.pi * jnp.arange(S5_STATE, dtype=f32), (DEPTH, S5_GROUPS, S5_STATE))
    ssd_dt = jnp.exp(jax.random.uniform(ks[18], (DEPTH, SSD_HEADS), f32, math.log(0.001), math.log(0.1)))
    return {
        'x': nrm(ks[0], (BATCH, SEQ, D_MODEL), 1.0),
        'norm_mix_g': gain(ks[1], (DEPTH, D_MODEL)),
        'w_in': nrm(ks[2], (DEPTH, D_MODEL, N_IN), D_MODEL ** -0.5),
        'attn_q_norm_g': gain(ks[3], (DEPTH, HEAD_DIM)),
        'attn_k_norm_g': gain(ks[4], (DEPTH, HEAD_DIM)),
        'idx_k_norm_g': gain(ks[5], (DEPTH, IDX_DIM)),
        's5_lambda_re': -0.5 + 0.01 * jax.random.normal(ks[6], (DEPTH, S5_GROUPS, S5_STATE), f32),
        's5_lambda_im': lam_im + 0.01 * jax.random.normal(ks[7], (DEPTH, S5_GROUPS, S5_STATE), f32),
        's5_log_step': jax.random.uniform(ks[8], (DEPTH, S5_GROUPS), f32, math.log(0.001), math.log(0.1)),
        's5_b_re': nrm(ks[9], (DEPTH, S5_GROUPS, S5_STATE, S5_GROUP_CH), (2 * S5_GROUP_CH) ** -0.5),
        's5_b_im': nrm(ks[10], (DEPTH, S5_GROUPS, S5_STATE, S5_GROUP_CH), (2 * S5_GROUP_CH) ** -0.5),
        's5_c_re': nrm(ks[11], (DEPTH, S5_GROUPS, S5_GROUP_CH, S5_STATE), S5_STATE ** -0.5),
        's5_c_im': nrm(ks[12], (DEPTH, S5_GROUPS, S5_GROUP_CH, S5_STATE), S5_STATE ** -0.5),
        's5_d': nrm(ks[13], (DEPTH, S5_WIDTH), 1.0),
        's5_glu_w': nrm(ks[14], (DEPTH, S5_WIDTH, S5_WIDTH), S5_WIDTH ** -0.5),
        's5_glu_b': nrm(ks[15], (DEPTH, S5_WIDTH), 0.01),
        'ssd_conv_w': nrm(ks[16], (DEPTH, SSD_CONV, SSD_XBC), SSD_CONV ** -0.5),
        'ssd_conv_b': nrm(ks[17], (DEPTH, SSD_XBC), 0.01),
        'ssd_dt_bias': ssd_dt + jnp.log(-jnp.expm1(-ssd_dt)),
        'ssd_a_log': jnp.log(jax.random.uniform(ks[19], (DEPTH, SSD_HEADS), f32, 1.0, 16.0)),
        'ssd_d': 1.0 + 0.01 * jax.random.normal(ks[20], (DEPTH, SSD_HEADS), f32),
        'ssd_norm_g': gain(ks[21], (DEPTH, SSD_WIDTH)),
        'w_out': nrm(ks[22], (DEPTH, D_MIX, D_MODEL), D_MIX ** -0.5),
        'norm_mlp_g': gain(ks[23], (DEPTH, D_MODEL)),
        'w_up': nrm(ks[24], (DEPTH, D_MODEL, D_FF), D_MODEL ** -0.5),
        'w_down': nrm(ks[25], (DEPTH, D_FF, D_MODEL), D_FF ** -0.5),
    }


def reference(x, norm_mix_g, w_in, attn_q_norm_g, attn_k_norm_g, idx_k_norm_g,
              s5_lambda_re, s5_lambda_im, s5_log_step, s5_b_re, s5_b_im, s5_c_re, s5_c_im,
              s5_d, s5_glu_w, s5_glu_b,
              ssd_conv_w, ssd_conv_b, ssd_dt_bias, ssd_a_log, ssd_d, ssd_norm_g,
              w_out, norm_mlp_g, w_up, w_down):
    cos, sin = _rope_tables(x.shape[1])
    for l in range(DEPTH):
        h = _rmsnorm(x, norm_mix_g[l])
        z = h @ w_in[l]
        q, k, v, qi, ki, wi, u5, z_ssd, xbc, dt_raw = _split(z, IN_SIZES)
        attn_out = _dsa_mixer(q, k, v, qi, ki, wi, attn_q_norm_g[l], attn_k_norm_g[l],
                              idx_k_norm_g[l], cos, sin)
        s5_out = _s5_mixer(u5, s5_lambda_re[l], s5_lambda_im[l], s5_log_step[l],
                           s5_b_re[l], s5_b_im[l], s5_c_re[l], s5_c_im[l],
                           s5_d[l], s5_glu_w[l], s5_glu_b[l])
        ssd_out = _ssd_mixer(z_ssd, xbc, dt_raw, ssd_conv_w[l], ssd_conv_b[l], ssd_dt_bias[l],
                             ssd_a_log[l], ssd_d[l], ssd_norm_g[l])
        mix = jnp.concatenate([attn_out, s5_out, ssd_out], axis=-1) @ w_out[l]
        x = x + mix.astype(x.dtype)
        h2 = _rmsnorm(x, norm_mlp_g[l])
        x = x + ((jax.nn.relu(h2 @ w_up[l]) ** 2) @ w_down[l]).astype(x.dtype)
    return x
```

```python
import numpy as np
import sys
from contextlib import ExitStack
import concourse.bass as bass
import concourse.mybir as mybir
from concourse.bass_utils import run_bass_kernel_spmd

F32 = mybir.dt.float32
BF16 = mybir.dt.bfloat16
AF = mybir.ActivationFunctionType
ALU = mybir.AluOpType
AX = mybir.AxisListType


class Sched:
    ENG = ('pe', 'act', 'dve', 'pool', 'sp')
    NDQ = 6

    def __init__(self, nc):
        self.nc = nc
        self.prog = {e: [] for e in self.ENG}
        self.cnt = {e: 0 for e in self.ENG}
        self.waited = {e: {} for e in self.ENG}
        self.lw = {}
        self.rd = {}
        self.dma_i = {e: 0 for e in self.ENG}
        self.dma_cnt = {}
        self.sems = {}
        self.stack = ExitStack()
        self.n_ops = 0

    def ctx(self):
        nc = self.nc
        for e in ('pe', 'act', 'dve', 'pool'):
            self.sems[e] = self.stack.enter_context(nc.semaphore('s_' + e))
        for q in ('sp', 'pool', 'act'):
            for j in range(self.NDQ):
                k = 'd%s%d' % (q, j)
                self.sems[k] = self.stack.enter_context(nc.semaphore('s_' + k))
                self.dma_cnt[k] = 0
        return self.stack

    def _nm(self, p):
        self.n_names = getattr(self, 'n_names', 0) + 1
        return '%s%d' % (p, self.n_names)

    def sb(self, shape, dtype, name=None):
        return self.stack.enter_context(self.nc.sbuf_tensor(self._nm('sb'), list(shape), dtype))

    def psum(self):
        return self.stack.enter_context(self.nc.psum_tensor(self._nm('ps'), [128, 512], F32))

    def psum_bf16(self):
        return self.stack.enter_context(self.nc.psum_tensor(self._nm('pb'), [128, 1024], BF16))

    def _deps(self, eng, reads, writes):
        deps = {}

        def add(tok):
            if tok is None:
                return
            k, v = tok
            if deps.get(k, 0) < v:
                deps[k] = v
        for b in reads:
            add(self.lw.get(b))
            if isinstance(b, str) and b.startswith('pb'):
                for k, v in self.rd.get(b, {}).items():
                    if k != eng:
                        add((k, v))
        for b in writes:
            add(self.lw.get(b))
            for k, v in self.rd.get(b, {}).items():
                add((k, v))
        waits = []
        w = self.waited[eng]
        for k, v in deps.items():
            if eng == 'pe' and k == 'pe':
                continue
            if w.get(k, 0) >= v:
                continue
            w[k] = v
            waits.append((k, v))
        return waits

    def _commit(self, tok, reads, writes):
        for b in writes:
            self.lw[b] = tok
            self.rd[b] = {}
        for b in reads:
            if b in writes:
                continue
            d = self.rd.setdefault(b, {})
            if d.get(tok[0], 0) < tok[1]:
                d[tok[0]] = tok[1]

    def op(self, eng, fn, reads=(), writes=(), selfsync=False):
        waits = self._deps(eng, reads, writes)
        if selfsync and self.cnt[eng] > 0 and self.waited[eng].get(eng, 0) < self.cnt[eng]:
            self.waited[eng][eng] = self.cnt[eng]
            waits.append((eng, self.cnt[eng]))
        self.cnt[eng] += 1
        tok = (eng, self.cnt[eng])
        self.prog[eng].append((waits, fn, (eng, 1), self._where()))
        self._commit(tok, reads, writes)
        self.n_ops += 1
        return tok

    def dma(self, q, out, in_, reads=(), writes=(), final=False, **kw):
        waits = self._deps(q, reads, writes)
        j = self.dma_i[q] % self.NDQ
        self.dma_i[q] += 1
        k = 'd%s%d' % (q, j)
        prev = 16 * self.dma_cnt[k]
        if prev > 0 and self.waited[q].get(k, 0) < prev:
            self.waited[q][k] = prev
            waits.append((k, prev))
        self.dma_cnt[k] += 1
        tok = (k, 16 * self.dma_cnt[k])
        self.prog[q].append((waits, lambda e: e.dma_start(out=out, in_=in_, **kw), (k, 16), self._where()))
        self._commit(tok, reads, writes)
        self.n_ops += 1
        return tok

    def _where(self):
        out = []
        f = sys._getframe(2)
        while f is not None and len(out) < 4:
            out.append(f.f_lineno)
            f = f.f_back
        return out

    def barrier(self):
        toks = {}
        for e in ('pe', 'act', 'dve', 'pool'):
            if self.cnt[e] > 0:
                toks[e] = self.cnt[e]
        for k, c in self.dma_cnt.items():
            if c > 0:
                toks[k] = 16 * c
        for e in self.ENG:
            w = self.waited[e]
            waits = []
            for k, v in toks.items():
                if k == e:
                    continue
                if w.get(k, 0) < v:
                    w[k] = v
                    waits.append((k, v))
            if waits:
                self.prog[e].append((waits, None, None, None))

    def _emit(self, eng, e):
        sems = self.sems
        for waits, fn, inc, where in self.prog[eng]:
            for k, v in waits:
                e.wait_ge(sems[k], v)
            if fn is not None:
                try:
                    ins = fn(e)
                except Exception:
                    print("EMIT ERROR at lines", where, file=sys.stderr)
                    raise
                ins.then_inc(sems[inc[0]], inc[1])

    def finish(self):
        self.barrier()
        nc = self.nc
        with nc.Block() as block:
            @block.sync
            def _(e):
                self._emit('sp', e)

            @block.tensor
            def _(e):
                self._emit('pe', e)

            @block.scalar
            def _(e):
                self._emit('act', e)

            @block.vector
            def _(e):
                self._emit('dve', e)

            @block.gpsimd
            def _(e):
                self._emit('pool', e)


D = 1024
DFF = 4096
EPS = 1e-6
NIT = 20
IDX_W_SCALE = (8 ** -0.5) * (64 ** -0.5)
TWO_PI = 6.283185307179586
NEG_BIG = -3.0e38


def _layout(items):
    off = {}
    o = 0
    for n, w in items:
        off[n] = (o, w)
        o += w
    return off, o


PP_ITEMS = [('gmix', 8), ('gmlp', 8), ('gq', 512), ('gk', 128), ('gki', 64), ('dtb', 4), ('alog', 4),
            ('dskip', 4), ('gssd', 2), ('convw', 2048), ('convb_row', 512), ('convb_col', 2),
            ('lr_row', 1024), ('li_row', 1024), ('ls_row', 1024),
            ('lrB', 128), ('liB', 128), ('lsB', 128), ('bre', 128), ('bim', 128),
            ('cre', 128), ('cim', 128), ('dcol', 2), ('glub', 2)]
PP, NPP = _layout(PP_ITEMS)
CST_ITEMS = [('ident', 128), ('U', 128), ('M1', 128), ('NEGsl4', 512), ('NEGq', 128), ('Sel', 128),
             ('ones', 128), ('n1col', 1), ('pow2', NIT), ('bmask', 512), ('cmask', 32)]
CST, NCST = _layout(CST_ITEMS)
C_Q, C_QI, C_KV, C_Z, C_U5, C_XBC = 0, 512, 1024, 1356, 1612, 1868
CB_Z = 1408


def host_consts(SQ):
    c = np.zeros((128, NCST), np.float32)

    def put(n, a):
        o, w = CST[n]
        c[:, o:o + w] = np.asarray(a, np.float32).reshape(128, w)
    i = np.arange(128)
    put('ident', np.eye(128))
    put('U', (i[:, None] <= i[None, :]))
    put('M1', (i[:, None] > i[None, :]))
    negsl = np.where(i[None, :] < i[:, None], -30000.0, 0.0)
    put('NEGsl4', np.tile(negsl, (1, 4)))
    put('NEGq', np.where(i[None, :] > i[:, None], NEG_BIG, 0.0))
    sel = np.zeros((128, 128))
    sel[127, :] = 1
    put('Sel', sel)
    put('ones', np.ones((128, 128)))
    put('n1col', (i + 1.0)[:, None])
    put('pow2', np.tile(2.0 ** -(np.arange(NIT) + 1.0), (128, 1)))
    bm = np.zeros((128, 8, 64))
    for g in range(8):
        bm[16 * g:16 * g + 16, g, :] = 1
    put('bmask', bm)
    cm = np.zeros((128, 2, 16))
    for g in range(2):
        cm[64 * g:64 * g + 64, g, :] = 1
    put('cmask', cm)
    pos = np.arange(SQ, dtype=np.float32)
    inv_freq = (np.float32(500000.0) ** (-np.arange(0, 16, 2, dtype=np.float32) / np.float32(16))).astype(np.float32)
    ang = (pos[:, None] * inv_freq[None, :]).astype(np.float32)
    cs = np.concatenate([np.cos(ang), np.sin(ang)], axis=1).astype(np.float32)
    return c, cs


_QH = np.concatenate([np.arange(64 * h, 64 * h + 64) for h in (0, 4, 1, 5, 2, 6, 3, 7)])
W_IN_PERM = np.concatenate([_QH, np.arange(768, 1280), np.arange(512, 640), np.arange(640, 768),
                            np.arange(1280, 1344), np.arange(1344, 1352), np.arange(2376, 2380),
                            np.arange(1608, 1864), np.arange(1352, 1608), np.arange(1864, 2376)])


def host_pp(inp, l):
    p = np.zeros((128, NPP), np.float32)

    def put(n, a):
        o, w = PP[n]
        p[:, o:o + w] = np.asarray(a, np.float32).reshape(128, w)

    def rows(v):
        v = np.asarray(v, np.float32).reshape(1, -1)
        return np.tile(v, (128, 1))
    put('gmix', inp['norm_mix_g'][l].reshape(8, 128).T)
    put('gmlp', inp['norm_mlp_g'][l].reshape(8, 128).T)
    put('gq', rows(np.tile(inp['attn_q_norm_g'][l], 8)))
    put('gk', rows(np.tile(inp['attn_k_norm_g'][l], 2)))
    put('gki', rows(inp['idx_k_norm_g'][l]))
    put('dtb', rows(inp['ssd_dt_bias'][l]))
    put('alog', rows(inp['ssd_a_log'][l]))
    put('dskip', rows(inp['ssd_d'][l]))
    put('gssd', inp['ssd_norm_g'][l].reshape(2, 128).T)
    put('convw', rows(inp['ssd_conv_w'][l].reshape(-1)))
    put('convb_row', rows(inp['ssd_conv_b'][l]))
    put('convb_col', inp['ssd_conv_b'][l][256:512].reshape(2, 128).T)
    put('lr_row', rows(inp['s5_lambda_re'][l].reshape(-1)))
    put('li_row', rows(inp['s5_lambda_im'][l].reshape(-1)))
    put('ls_row', rows(np.repeat(inp['s5_log_step'][l], 64)))

    def gh(a):
        a = np.asarray(a, np.float32).reshape(2, 8, 1, 64)
        a = np.broadcast_to(a, (2, 8, 16, 64))
        return a.transpose(1, 2, 0, 3).reshape(128, 128)
    put('lrB', gh(inp['s5_lambda_re'][l]))
    put('liB', gh(inp['s5_lambda_im'][l]))
    put('lsB', gh(np.repeat(inp['s5_log_step'][l][:, None], 64, axis=1)))

    def bl(a):
        a = np.asarray(a, np.float32).reshape(2, 8, 64, 16)
        return a.transpose(1, 3, 0, 2).reshape(128, 128)
    put('bre', bl(inp['s5_b_re'][l]))
    put('bim', bl(inp['s5_b_im'][l]))

    def cl(a):
        a = np.asarray(a, np.float32).reshape(8, 2, 16, 64)
        return a.transpose(1, 3, 0, 2).reshape(128, 128)
    put('cre', cl(inp['s5_c_re'][l]))
    put('cim', cl(inp['s5_c_im'][l]))
    put('dcol', inp['s5_d'][l].reshape(2, 128).T)
    put('glub', inp['s5_glu_b'][l].reshape(2, 128).T)
    return p


class Rot:
    def __init__(self, aps, name):
        self.aps, self.i = aps, 0
        self.keys = list(name) if isinstance(name, (list, tuple)) else ['%s%d' % (name, i) for i in range(len(aps))]

    def next(self):
        i = self.i % len(self.aps)
        self.i += 1
        return self.aps[i], self.keys[i]

    def prev(self):
        i = (self.i - 2) % len(self.aps)
        return self.aps[i], self.keys[i]


def build(SQ, NL, debug=False, stop=None):
    NT = SQ // 128
    NQB = SQ // 512
    TOPK = min(256, SQ // 4)
    nc = bass.Bass("TRN2", target_bir_lowering=False)

    def din(name, shape):
        return nc.dram_tensor(name, list(shape), F32, kind="ExternalInput").ap()

    def dscr(name, shape, dt=BF16):
        return nc.dram_tensor(name, list(shape), dt, kind=("ExternalOutput" if debug else "Internal")).ap()
    x_d = din("x", [SQ, D])
    win_d = din("w_in", [NL, D, 2380])
    wout_d = din("w_out", [NL, D, D])
    wup_d = din("w_up", [NL, D, DFF])
    wdn_d = din("w_down", [NL, DFF, D])
    glu_d = din("glu_w", [NL, 256, 256])
    pp_d = din("pp", [NL, 128, NPP])
    cst_d = din("cst", [128, NCST])
    cs_d = din("cs", [SQ, 16])
    out_d = nc.dram_tensor("out", [SQ, D], F32, kind="ExternalOutput").ap()
    xb_d = nc.dram_tensor("xb", [SQ, D], F32, kind="Internal").ap()
    qT_s = dscr("qT_s", [4, 128, SQ])
    qiT_s = dscr("qiT_s", [4, 128, SQ])
    kT_s = dscr("kT_s", [128, SQ])
    kiT_s = dscr("kiT_s", [64, SQ])
    v_s = dscr("v_s", [SQ, 130])
    sg_s = dscr("sg_s", [SQ, 8], F32)
    u5T_s = dscr("u5T_s", [2, 128, SQ])
    xsb_s = dscr("xsb_s", [SQ, 384])
    bcT_s = dscr("bcT_s", [2, 128, SQ])
    zs_s = dscr("zs_s", [SQ, 256], F32)
    dt_s = dscr("dt_s", [SQ, 32], F32)
    catT_s = dscr("catT_s", [8, 128, SQ])
    aT_s = nc.dram_tensor("aT_s", [32, 128, SQ], BF16, kind="Internal").ap()
    tk_s = dscr("tk_s", [SQ, 4], F32)

    S = Sched(nc)
    with S.ctx():
        ARENA = 52736
        arena = S.sb([128, ARENA], F32)
        PB = [S.psum() for _ in range(8)]
        PBk = ['pb%d' % i for i in range(8)]
        st = {'off': 0}

        def A(shape, dt=F32):
            n = 1
            for s_ in shape[1:]:
                n *= s_
            cols = n if dt == F32 else (n + 1) // 2
            o = st['off']
            st['off'] += cols
            assert st['off'] <= ARENA, "arena overflow %d" % st['off']
            ap = arena[:, o:o + cols]
            if dt != F32:
                ap = ap.bitcast(dt)[:, 0:n]
            if len(shape) == 3:
                ap = ap.rearrange("p (a b) -> p a b", a=shape[1])
            elif len(shape) == 4:
                ap = ap.rearrange("p (a b c) -> p a b c", a=shape[1], b=shape[2])
            if shape[0] != 128:
                ap = ap[0:shape[0]]
            return ap

        def mm(out, lhsT, rhs, start, stop, r, w, selfsync=False):
            S.op('pe', lambda e: e.matmul(out, lhsT, rhs, start=start, stop=stop), r, w, selfsync=selfsync)

        def tr(out, in_, ident, r, w):
            S.op('pe', lambda e: e.transpose(out, in_, ident), r, w)

        def act(out, in_, func, r, w, **kw):
            S.op('act', lambda e: e.activation(out, in_, func, **kw), r, w)

        def cp(eng, out, in_, r, w):
            if eng == 'act':
                S.op('act', lambda e: e.copy(out, in_), r, w)
            else:
                S.op(eng, lambda e: e.tensor_copy(out, in_), r, w)

        def tt(eng, out, in0, in1, op, r, w):
            S.op(eng, lambda e: e.tensor_tensor(out, in0, in1, op), r, w)

        def ts(eng, out, in0, s1, s2, op0, op1, r, w, **kw):
            if op1 is None:
                S.op(eng, lambda e: e.tensor_scalar(out, in0, s1, None, op0=op0, **kw), r, w)
            else:
                S.op(eng, lambda e: e.tensor_scalar(out, in0, s1, s2, op0=op0, op1=op1, **kw), r, w)

        def stt(eng, out, in0, sc, in1, op0, op1, r, w):
            S.op('dve', lambda e: e.scalar_tensor_tensor(out, in0, sc, in1, op0=op0, op1=op1), r, w)

        def memset(eng, out, val, w):
            S.op(eng, lambda e: e.memset(out, val), (), w)

        def bview(i, n=1024):
            return PB[i].bitcast(BF16)[:, 0:n]

        cst = A([128, NCST])
        S.dma('sp', cst, cst_d[:, :], ['cst_d'], ['cst'])

        def C(n):
            o, w = CST[n]
            return cst[:, o:o + w]
        ident_b = A([128, 128], BF16)
        U_b = A([128, 128], BF16)
        NEGsl4_b = A([128, 512], BF16)
        ones_b = A([128, 128], BF16)
        cp('dve', ident_b, C('ident'), ['cst'], ['ident_b'])
        cp('dve', U_b, C('U'), ['cst'], ['U_b'])
        cp('dve', NEGsl4_b, C('NEGsl4'), ['cst'], ['NEGsl4_b'])
        cp('dve', ones_b, C('ones'), ['cst'], ['ones_b'])
        negn1 = A([128, 1])
        ts('dve', negn1, C('n1col'), -1.0, None, ALU.mult, None, ['cst'], ['negn1'])
        KC = ['cst', 'ident_b', 'U_b', 'NEGsl4_b', 'ones_b', 'negn1']
        P_BASE = st['off']

        def new_phase():
            S.barrier()
            st['off'] = P_BASE

        def rstd_inplace(ap, key, scale):
            ts('dve', ap, ap, scale, EPS, ALU.mult, ALU.add, [key], [key])
            act(ap, ap, AF.Sqrt, [key], [key])
            S.op('dve', lambda e: e.reciprocal(ap, ap), [key], [key])

        def sinred(out, ang, off, n, tmp, key, okey=None):
            okey = okey or (key + 'out')
            r_, k_, f_ = tmp
            ts('dve', r_, ang, 1.0 / TWO_PI, off, ALU.mult, ALU.add, [key + 'ang'], [key + 'r'])
            cp('dve', k_.bitcast(mybir.dt.int32), r_, [key + 'r'], [key + 'k'])
            cp('dve', f_, k_.bitcast(mybir.dt.int32), [key + 'k'], [key + 'f'])
            tt('dve', r_, r_, f_, ALU.subtract, [key + 'r', key + 'f'], [key + 'r'])
            stt('dve', f_, r_, 0.5, r_, ALU.is_gt, ALU.subtract, [key + 'r'], [key + 'f'])
            stt('dve', r_, f_, 0.5, f_, ALU.is_gt, ALU.subtract, [key + 'f'], [key + 'r'])
            act(out, r_, AF.Sin, [key + 'r'], [okey], scale=TWO_PI)

        for l in range(NL):
            xsrc = x_d if l == 0 else xb_d
            xdst = out_d if l == NL - 1 else xb_d
            xin_key = (lambda t: 'xin%d' % t)

            def PPd(n, l=l):
                o, w = PP[n]
                return pp_d[l, :, o:o + w]

            new_phase()
            Win_b = A([128, 8, 1664], BF16)
            Wu5_b = A([128, 8, 256], BF16)
            Wc_b = A([128, 4, 8, 512], BF16)
            ppa = A([128, 8 + 8 + 512 + 128 + 64 + 4 + 4 + 4 + 2])
            NPA = 8 + 8 + 512 + 128 + 64 + 4 + 4 + 4 + 2
            S.dma('sp', ppa, pp_d[l, :, 0:NPA], ['pp_d'], ['ppa'])
            convw = A([128, 4, 512])
            S.dma('sp', convw, PPd('convw').rearrange("p (a b) -> p a b", a=4), ['pp_d'], ['convw'])
            cbrow = A([128, 512])
            S.dma('sp', cbrow, PPd('convb_row'), ['pp_d'], ['cbrow'])
            cbcol = A([128, 2])
            S.dma('sp', cbcol, PPd('convb_col'), ['pp_d'], ['cbcol'])
            cbrow_b = A([128, 384], BF16)
            cp('dve', cbrow_b, cbrow[:, 0:384], ['cbrow'], ['cbrow_b'])

            def PA(n):
                o, w = PP[n]
                return ppa[:, o:o + w]
            aneg = A([128, 4])
            act(aneg, PA('alog'), AF.Exp, ['ppa'], ['aneg'])
            ts('dve', aneg, aneg, -1.0, None, ALU.mult, None, ['aneg'], ['aneg'])
            stg = Rot([A([128, 2380]), A([128, 2380])], 'stg')
            for c in range(8):
                sg_, sk = stg.next()
                S.dma('sp' if c % 2 == 0 else 'pool', sg_, win_d[l, c * 128:(c + 1) * 128, :], ['win_d'], [sk])
                gm = PA('gmix')[:, c:c + 1]
                ts('dve', Win_b[:, c, 0:1356], sg_[:, 0:1356], gm, None, ALU.mult, None, [sk, 'ppa'], ['Win_b'])
                ts('dve', Win_b[:, c, CB_Z:CB_Z + 256], sg_[:, C_Z:C_Z + 256], gm, None, ALU.mult, None, [sk, 'ppa'], ['Win_b'])
                ts('pool', Wu5_b[:, c, :], sg_[:, C_U5:C_U5 + 256], gm, None, ALU.mult, None, [sk, 'ppa'], ['Wu5_b'])
                for tap in range(4):
                    stt('dve' if tap % 2 == 0 else 'pool', Wc_b[:, tap, c, :], sg_[:, C_XBC:C_XBC + 512], gm, convw[:, tap, :],
                        ALU.mult, ALU.mult, [sk, 'ppa', 'convw'], ['Wc_b'])
            xrot = Rot([A([128, D]), A([128, D])], 'xt')
            hrot = Rot([A([128, 8, 128], BF16), A([128, 8, 128], BF16)], 'hT')
            hsh = [A([128, 8, 128], BF16) for _ in range(3)]
            hb = A([128, D], BF16)
            junkA = A([128, D])
            ss = A([128, 1])
            sq = A([128, 512])
            qn = A([128, 512])
            qb = A([128, 512], BF16)
            st8 = A([128, 8])
            rtmp = [A([128, 64]) for _ in range(4)]
            csrot = Rot([A([128, 16]), A([128, 16])], 'cs')
            qTt = Rot([A([128, 4, 128], BF16), A([128, 4, 128], BF16)], 'qTt')
            qiTt = Rot([A([128, 4, 128], BF16), A([128, 4, 128], BF16)], 'qiTt')
            kTt = Rot([A([128, 128], BF16), A([128, 128], BF16)], 'kTt')
            kiTt = Rot([A([64, 128], BF16), A([64, 128], BF16)], 'kiTt')
            vbt = Rot([A([128, 2, 65], BF16), A([128, 2, 65], BF16)], 'vbt')
            for v_, _k in zip(vbt.aps, ('vbt0', 'vbt1')):
                memset('pool', v_, 1.0, [_k])
            sgt = Rot([A([128, 8]), A([128, 8])], 'sgt')
            wabs = A([128, 8])
            wneg = A([128, 8])
            dtt = Rot([A([128, 32]), A([128, 32])], 'dtt')
            for d__, k__ in zip(dtt.aps, ('dtt0', 'dtt1')):
                memset('pool', d__, 0.0, [k__])
            dtmp = [A([128, 4]) for _ in range(3)]
            zst = Rot([A([128, 256]), A([128, 256])], 'zst')
            u5t = Rot([A([128, 2, 128], BF16), A([128, 2, 128], BF16)], 'u5t')
            xsbt = Rot([A([128, 384], BF16), A([128, 384], BF16)], 'xsbt')
            bct = Rot([A([128, 2, 128], BF16), A([128, 2, 128], BF16)], 'bct')

            def rope(src3, dst3, H, cst_, csk, sk, dk, eng='pool'):
                cb = cst_[:, 0:8].unsqueeze(1).to_broadcast([128, H, 8])
                sb_ = cst_[:, 8:16].unsqueeze(1).to_broadcast([128, H, 8])
                x1 = src3[:, :, 0:8]
                x2 = src3[:, :, 8:16]
                t = [r_[:, 0:H * 8].rearrange("p (h j) -> p h j", h=H) for r_ in rtmp]
                tt(eng, t[0], x1, cb, ALU.mult, [sk, csk], ['rt0'])
                tt(eng, t[1], x2, sb_, ALU.mult, [sk, csk], ['rt1'])
                tt(eng, dst3[:, :, 0:8], t[0], t[1], ALU.subtract, ['rt0', 'rt1'], [dk])
                tt(eng, t[2], x2, cb, ALU.mult, [sk, csk], ['rt2'])
                tt(eng, t[3], x1, sb_, ALU.mult, [sk, csk], ['rt3'])
                tt(eng, dst3[:, :, 8:16], t[2], t[3], ALU.add, ['rt2', 'rt3'], [dk])

            def headnorm(ps_ap, psk, H, g_ap):
                n = H * 64
                act(sq[:, 0:n], ps_ap, AF.Square, [psk], ['sq'])
                S.op('dve', lambda e: e.tensor_reduce(st8[:, 0:H], sq[:, 0:n].rearrange("p (h d) -> p h d", h=H), AX.X, ALU.add), ['sq'], ['st8'])
                rstd_inplace(st8[:, 0:H], 'st8', 1.0 / 64)
                tt('dve', qn[:, 0:n].rearrange("p (h d) -> p h d", h=H), ps_ap.rearrange("p (h d) -> p h d", h=H),
                   st8[:, 0:H].unsqueeze(2).to_broadcast([128, H, 64]), ALU.mult, [psk, 'st8'], ['qn'])
                tt('pool', qn[:, 0:n], qn[:, 0:n], g_ap, ALU.mult, ['qn', 'ppa'], ['qn'])

            for t in range(NT if stop != 'A0' else 0):
                tok = slice(t * 128, (t + 1) * 128)
                xt, xk = xrot.next()
                S.dma('sp', xt, xsrc[tok, :], [xin_key(t)], [xk])
                cst_, csk = csrot.next()
                S.dma('pool', cst_, cs_d[tok, :], ['cs_d'], [csk])
                memset('pool', ss, 0.0, ['ss'])
                act(junkA, xt, AF.Square, [xk], ['junkA', 'ss'], accum_out=ss)
                rstd_inplace(ss, 'ss', 1.0 / D)
                ts('dve', hb, xt, ss, None, ALU.mult, None, [xk, 'ss'], ['hb'])
                hT, hk = hrot.next()
                pT = bview(7)
                for c in range(8):
                    tr(pT[:, c * 128:(c + 1) * 128], hb[:, c * 128:(c + 1) * 128], ident_b, ['hb', 'ident_b'], [PBk[7]])
                cp('act', hT, pT.rearrange("p (c t) -> p c t", c=8), [PBk[7]], [hk])
                hp, hpk = hrot.prev()
                for tap in range(3):
                    nh = 3 - tap
                    e_ = 'pool' if tap % 2 == 0 else 'act'
                    if t == 0:
                        memset('pool', hsh[tap][:, :, 0:nh], 0.0, ['hsh%d' % tap])
                    else:
                        cp('pool', hsh[tap][:, :, 0:nh], hp[:, :, 128 - nh:128], [hpk], ['hsh%d' % tap])
                    cp(e_, hsh[tap][:, :, nh:128], hT[:, :, 0:128 - nh], [hk], ['hsh%d' % tap])
                H = hsh + [hT]
                Hk = ['hsh0', 'hsh1', 'hsh2', hk]
                hc = [hT[:, c, :] for c in range(8)]
                if stop == 'A1':
                    continue
                for c in range(8):
                    mm(PB[2][:, 0:332], hc[c], Win_b[:, c, C_KV:C_KV + 332], c == 0, c == 7, [hk, 'Win_b'], [PBk[2]])
                ts('dve', wabs, PB[2][:, 320:328], IDX_W_SCALE, None, ALU.mult, None, [PBk[2]], ['wabs'])
                ts('dve', wneg, PB[2][:, 320:328], -IDX_W_SCALE, None, ALU.mult, None, [PBk[2]], ['wneg'])
                tt('dve', wabs, wabs, wneg, ALU.max, ['wabs', 'wneg'], ['wabs'])
                sgx, sgk = sgt.next()
                ts('dve', sgx, PB[2][:, 320:328], 0.0, 2.0, ALU.is_ge, ALU.mult, [PBk[2]], [sgk])
                ts('dve', sgx, sgx, -1.0, None, ALU.add, None, [sgk], [sgk])
                S.dma('sp', sg_s[tok, :], sgx, [sgk], ['sg_s%d' % t])
                dtx, dtk = dtt.next()
                tt('dve', dtmp[0], PB[2][:, 328:332], PA('dtb'), ALU.add, [PBk[2], 'ppa'], ['dtmp0'])
                ts('dve', dtmp[2], dtmp[0], -1.0, None, ALU.mult, None, ['dtmp0'], ['dtmp2'])
                tt('dve', dtmp[1], dtmp[0], dtmp[2], ALU.max, ['dtmp0', 'dtmp2'], ['dtmp1'])
                act(dtmp[1], dtmp[1], AF.Exp, ['dtmp1'], ['dtmp1'], scale=-1.0)
                act(dtmp[1], dtmp[1], AF.Ln, ['dtmp1'], ['dtmp1'], bias=C('ones')[:, 0:1])
                stt('dve', dtx[:, 0:4], dtmp[0], 0.0, dtmp[1], ALU.max, ALU.add, ['dtmp0', 'dtmp1'], [dtk])
                tt('dve', dtx[:, 16:20], dtx[:, 0:4], aneg, ALU.mult, [dtk, 'aneg'], [dtk])
                S.dma('sp', dt_s[tok, :], dtx, [dtk], ['dt_s%d' % t])
                vb, vk = vbt.next()
                cp('act', vb[:, :, 0:64], PB[2][:, 128:256].rearrange("p (g d) -> p g d", g=2), [PBk[2]], [vk])
                S.dma('sp', v_s[tok, :], vb.rearrange("p g d -> p (g d)"), [vk], ['v_s%d' % t])
                headnorm(PB[2][:, 0:128], PBk[2], 2, PA('gk'))
                cp('act', qb[:, 0:128], qn[:, 0:128], ['qn'], ['qb'])
                rope(qn[:, 0:128].rearrange("p (h d) -> p h d", h=2), qb[:, 0:128].rearrange("p (h d) -> p h d", h=2), 2, cst_, csk, 'qn', 'qb')
                pk = PB[5].bitcast(BF16)[:, 512:640]
                tr(pk, qb[:, 0:128], ident_b, ['qb', 'ident_b'], [PBk[5]])
                kx, kk = kTt.next()
                cp('act', kx, pk, [PBk[5]], [kk])
                S.dma('sp', kT_s[:, tok], kx, [kk], ['kT_s%d' % t])
                headnorm(PB[2][:, 256:320], PBk[2], 1, PA('gki'))
                cp('act', qb[:, 0:64], qn[:, 0:64], ['qn'], ['qb'])
                rope(qn[:, 0:64].rearrange("p (h d) -> p h d", h=1), qb[:, 0:64].rearrange("p (h d) -> p h d", h=1), 1, cst_, csk, 'qn', 'qb')
                pki = PB[5].bitcast(BF16)[0:64, 640:768]
                tr(pki, qb[:, 0:64], ident_b, ['qb', 'ident_b'], [PBk[5]])
                kix, kik = kiTt.next()
                cp('act', kix, pki, [PBk[5]], [kik])
                S.dma('sp', kiT_s[:, tok], kix, [kik], ['kiT_s%d' % t])
                if stop == 'A2':
                    continue
                for c in range(8):
                    mm(PB[0][:, 0:512], hc[c], Win_b[:, c, C_Q:C_Q + 512], c == 0, c == 7, [hk, 'Win_b'], [PBk[0]])
                headnorm(PB[0][:, 0:512], PBk[0], 8, PA('gq'))
                cp('act', qb, qn, ['qn'], ['qb'])
                rope(qn.rearrange("p (h d) -> p h d", h=8), qb.rearrange("p (h d) -> p h d", h=8), 8, cst_, csk, 'qn', 'qb')
                pq = bview(6)
                for j in range(4):
                    tr(pq[:, j * 128:(j + 1) * 128], qb[:, j * 128:(j + 1) * 128], ident_b, ['qb', 'ident_b'], [PBk[6]])
                qx, qk_ = qTt.next()
                cp('act', qx, pq[:, 0:512].rearrange("p (j t) -> p j t", j=4), [PBk[6]], [qk_])
                S.dma('pool', qT_s[:, :, tok].rearrange("j p t -> p j t"), qx, [qk_], ['qT_s%d' % t])
                for c in range(8):
                    mm(PB[1][:, 0:512], hc[c], Win_b[:, c, C_QI:C_QI + 512], c == 0, c == 7, [hk, 'Win_b'], [PBk[1]])
                tt('dve', qn.rearrange("p (h d) -> p h d", h=8), PB[1][:, 0:512].rearrange("p (h d) -> p h d", h=8),
                   wabs.unsqueeze(2).to_broadcast([128, 8, 64]), ALU.mult, [PBk[1], 'wabs'], ['qn'])
                cp('act', qb, qn, ['qn'], ['qb'])
                rope(qn.rearrange("p (h d) -> p h d", h=8), qb.rearrange("p (h d) -> p h d", h=8), 8, cst_, csk, 'qn', 'qb')
                for j in range(4):
                    tr(pq[:, 512 + j * 128:512 + (j + 1) * 128], qb[:, j * 128:(j + 1) * 128], ident_b, ['qb', 'ident_b'], [PBk[6]])
                qix, qik = qiTt.next()
                cp('act', qix, pq[:, 512:1024].rearrange("p (j t) -> p j t", j=4), [PBk[6]], [qik])
                S.dma('pool', qiT_s[:, :, tok].rearrange("j p t -> p j t"), qix, [qik], ['qiT_s%d' % t])
                if stop == 'A3':
                    continue
                if stop != 'A4b':
                    for c in range(8):
                        mm(PB[3][:, 0:256], hc[c], Win_b[:, c, CB_Z:CB_Z + 256], c == 0, c == 7, [hk, 'Win_b'], [PBk[3]])
                    zx, zk = zst.next()
                    if stop != 'A4a1':
                        cp('act', zx, PB[3][:, 0:256], [PBk[3]], [zk])
                        if stop != 'A4a2':
                            S.dma('pool' if stop == 'A4a3' else 'sp', zs_s[tok, :], zx, [zk], ['zs_s%d' % t])
                if stop in ('A4a', 'A4a1', 'A4a2', 'A4a3'):
                    continue
                for cc in range(2):
                    for c in range(8):
                        mm(PB[3][:, 256 + cc * 128:256 + (cc + 1) * 128], Wu5_b[:, c, cc * 128:(cc + 1) * 128], hc[c], c == 0, c == 7,
                           [hk, 'Wu5_b'], [PBk[3]])
                ux, uk = u5t.next()
                cp('act', ux, PB[3][:, 256:512].rearrange("p (c t) -> p c t", c=2), [PBk[3]], [uk])
                S.dma('pool', u5T_s[:, :, tok].rearrange("c p t -> p c t"), ux, [uk], ['u5T_s%d' % t])
                if stop in ('A4', 'A4b'):
                    continue
                n_ = 0
                for tap in range(4):
                    for c in range(8):
                        mm(PB[4][:, 0:384], H[tap][:, c, :], Wc_b[:, tap, c, 0:384], n_ == 0, False, [Hk[tap], 'Wc_b'], [PBk[4]])
                        n_ += 1
                mm(PB[4][:, 0:384], ones_b[0:1, :], cbrow_b[0:1, :], False, True, ['ones_b', 'cbrow_b'], [PBk[4]])
                xx, xsk = xsbt.next()
                act(xx, PB[4][:, 0:384], AF.Silu, [PBk[4]], [xsk])
                S.dma('sp', xsb_s[tok, :], xx, [xsk], ['xsb_s%d' % t])
                if stop == 'A5':
                    continue
                for cc in range(2):
                    n_ = 0
                    for tap in range(4):
                        for c in range(8):
                            mm(PB[5][:, cc * 128:(cc + 1) * 128], Wc_b[:, tap, c, 256 + cc * 128:256 + (cc + 1) * 128], H[tap][:, c, :],
                               n_ == 0, n_ == 31, [Hk[tap], 'Wc_b'], [PBk[5]])
                            n_ += 1
                bx, bk = bct.next()
                for cc in range(2):
                    act(bx[:, cc, :], PB[5][:, cc * 128:(cc + 1) * 128], AF.Silu, [PBk[5], 'cbcol'], [bk], bias=cbcol[:, cc:cc + 1])
                S.dma('pool', bcT_s[:, :, tok].rearrange("c p t -> p c t"), bx, [bk], ['bcT_s%d' % t])

            if stop is not None and stop.startswith('A'):
                break
            phase_s5(S, locals())
            if stop in ('s5', 's5a', 's5b'):
                break
            phase_ssd(S, locals())
            if stop is not None and stop.startswith('ssd'):
                break
            phase_attn(S, locals())
            if stop == 'attn':
                break
            phase_ffn(S, locals())
        S.finish()
    return nc


class _NS:
    def __init__(self, d):
        self.__dict__.update(d)


def phase_s5(S, Ld):
    L = _NS(Ld)
    A, mm, tr, act, cp, tt, ts, stt, memset, bview = L.A, L.mm, L.tr, L.act, L.cp, L.tt, L.ts, L.stt, L.memset, L.bview
    PB, PBk, C, PPd, l, NT = L.PB, L.PBk, L.C, L.PPd, L.l, L.NT
    ident_b, U_b = L.ident_b, L.U_b
    L.new_phase()
    lr = A([128, 1024]); li = A([128, 1024]); ls = A([128, 1024])
    S.dma('sp', lr, PPd('lr_row'), ['pp_d'], ['lr'])
    S.dma('pool', li, PPd('li_row'), ['pp_d'], ['li'])
    S.dma('sp', ls, PPd('ls_row'), ['pp_d'], ['ls'])
    act(ls, ls, AF.Exp, ['ls'], ['ls'])
    tt('dve', lr, lr, ls, ALU.mult, ['lr', 'ls'], ['lr'])
    tt('dve', li, li, ls, ALU.mult, ['li', 'ls'], ['li'])
    mag1 = A([128, 1024]); mag2 = A([128, 1024])
    act(mag2, lr, AF.Exp, ['lr', 'cst'], ['mag2'], scale=C('n1col'))
    act(mag1, lr, AF.Exp, ['lr', 'negn1'], ['mag1'], scale=L.negn1)
    ang = A([128, 1024])
    ts('dve', ang, li, C('n1col'), None, ALU.mult, None, ['li', 'cst'], ['Tang'])
    tmp = [A([128, 1024]) for _ in range(3)]
    sinT = A([128, 1024]); cosT = A([128, 1024])
    L.sinred(sinT, ang, 16.0, 1024, tmp, 'T', 'Tsin')
    L.sinred(cosT, ang, 16.25, 1024, tmp, 'T', 'Tout')
    T1re = A([128, 1024]); T1im = A([128, 1024]); T2re = A([128, 1024]); T2im = A([128, 1024])
    tt('dve', T2re, mag2, cosT, ALU.mult, ['mag2', 'Tout'], ['T2'])
    tt('dve', T2im, mag2, sinT, ALU.mult, ['mag2', 'Tsin'], ['T2'])
    tt('dve', T1re, mag1, cosT, ALU.mult, ['mag1', 'Tout'], ['T1'])
    stt('dve', T1im, mag1, -1.0, sinT, ALU.mult, ALU.mult, ['mag1', 'Tsin'], ['T1'])
    pb_ = A([128, 7 * 128])
    S.dma('sp', pb_, L.pp_d[l, :, PP['lrB'][0]:PP['lrB'][0] + 7 * 128], ['pp_d'], ['pb_'])
    lrB, liB, lsB, bre, bim, cre, cim = [pb_[:, i * 128:(i + 1) * 128] for i in range(7)]
    w = [A([128, 128]) for _ in range(12)]
    act(w[0], lsB, AF.Exp, ['pb_'], ['w0'])
    tt('dve', w[1], lrB, w[0], ALU.mult, ['pb_', 'w0'], ['w1'])
    tt('dve', w[2], liB, w[0], ALU.mult, ['pb_', 'w0'], ['Bang'])
    act(w[1], w[1], AF.Exp, ['w1'], ['w1'])
    L.sinred(w[3], w[2], 16.0, 128, [w[4], w[5], w[6]], 'B')
    S.op('dve', lambda e: e.tensor_copy(w[7], w[3]), ['Bout'], ['Bsin'])
    L.sinred(w[3], w[2], 16.25, 128, [w[4], w[5], w[6]], 'B')
    tt('dve', w[4], w[1], w[3], ALU.mult, ['w1', 'Bout'], ['abre'])
    tt('dve', w[5], w[1], w[7], ALU.mult, ['w1', 'Bsin'], ['abim'])
    ts('dve', w[4], w[4], -1.0, None, ALU.add, None, ['abre'], ['abre'])
    tt('dve', w[6], lrB, lrB, ALU.mult, ['pb_'], ['w6'])
    tt('dve', w[8], liB, liB, ALU.mult, ['pb_'], ['w8'])
    tt('dve', w[6], w[6], w[8], ALU.add, ['w6', 'w8'], ['w6'])
    S.op('dve', lambda e: e.reciprocal(w[6], w[6]), ['w6'], ['w6'])
    tt('dve', w[8], w[4], lrB, ALU.mult, ['abre', 'pb_'], ['w8'])
    tt('dve', w[9], w[5], liB, ALU.mult, ['abim', 'pb_'], ['w9'])
    tt('dve', w[8], w[8], w[9], ALU.add, ['w8', 'w9'], ['w8'])
    tt('dve', w[8], w[8], w[6], ALU.mult, ['w8', 'w6'], ['cr'])
    tt('dve', w[9], w[5], lrB, ALU.mult, ['abim', 'pb_'], ['w9'])
    tt('dve', w[10], w[4], liB, ALU.mult, ['abre', 'pb_'], ['w10'])
    tt('dve', w[9], w[9], w[10], ALU.subtract, ['w9', 'w10'], ['w9'])
    tt('dve', w[9], w[9], w[6], ALU.mult, ['w9', 'w6'], ['ci'])
    tt('dve', w[0], w[8], bre, ALU.mult, ['cr', 'pb_'], ['w0'])
    tt('dve', w[1], w[9], bim, ALU.mult, ['ci', 'pb_'], ['w1'])
    tt('dve', w[0], w[0], w[1], ALU.subtract, ['w0', 'w1'], ['bbre'])
    tt('dve', w[2], w[8], bim, ALU.mult, ['cr', 'pb_'], ['w2'])
    tt('dve', w[3], w[9], bre, ALU.mult, ['ci', 'pb_'], ['w3'])
    tt('dve', w[2], w[2], w[3], ALU.add, ['w2', 'w3'], ['bbim'])
    Bblk = A([128, 4, 512], BF16)
    bmask = C('bmask').rearrange("p (g q) -> p g q", g=8)
    for part, (bb, bbk) in enumerate(((w[0], 'bbre'), (w[2], 'bbim'))):
        for c in range(2):
            tt('dve', Bblk[:, part * 2 + c, :].rearrange("p (g q) -> p g q", g=8), bmask,
               bb[:, c * 64:(c + 1) * 64].unsqueeze(1).to_broadcast([128, 8, 64]), ALU.mult, [bbk, 'cst'], ['Bblk'])
    Cblk = A([128, 16, 32], BF16)
    cmask = C('cmask').rearrange("p (g h) -> p g h", g=2)
    cre3 = cre.rearrange("p (j h) -> p j h", j=8)
    cim3 = cim.rearrange("p (j h) -> p j h", j=8)
    for g2 in range(2):
        cmb = cmask[:, g2, :].unsqueeze(1).to_broadcast([128, 8, 16])
        tt('dve', Cblk[:, 0:8, 16 * g2:16 * (g2 + 1)], cre3, cmb, ALU.mult, ['pb_', 'cst'], ['Cblk'])
        stt('dve', Cblk[:, 8:16, 16 * g2:16 * (g2 + 1)], cim3, -1.0, cmb, ALU.mult, ALU.mult, ['pb_', 'cst'], ['Cblk'])
    dg2 = A([128, 4]); S.dma('sp', dg2, L.pp_d[l, :, PP['dcol'][0]:PP['dcol'][0] + 4], ['pp_d'], ['dg2'])
    dgd = A([128, 2, 128], BF16)
    for c in range(2):
        ts('dve', dgd[:, c, :], C('ident'), dg2[:, c:c + 1], None, ALU.mult, None, ['cst', 'dg2'], ['dgd'])
    glf = A([128, 2, 256])
    S.dma('sp', glf, L.glu_d[l].rearrange("(c p) n -> p c n", p=128), ['glu_d'], ['glf'])
    glb = A([128, 2, 256], BF16)
    cp('dve', glb, glf, ['glf'], ['glb'])
    uT = Rot([A([128, 2, 128], BF16), A([128, 2, 128], BF16)], 'uT')
    V = A([128, 2048], BF16)
    ta = [A([128, 512]) for _ in range(4)]
    X = Rot([A([128, 2048]), A([128, 2048])], 'X')
    Xb = A([128, 2048], BF16)
    XT = A([128, 16, 128], BF16)
    y2b = A([128, 256], BF16)
    y2T = A([128, 2, 128], BF16)
    sgm = A([128, 2, 128])
    s5o = Rot([A([128, 2, 128], BF16), A([128, 2, 128], BF16)], 's5o')
    Ts = {'T1re': T1re, 'T1im': T1im, 'T2re': T2re, 'T2im': T2im}

    def cmul(re_ps, im_ps, rk, ik, Tre, Tim, Tk, j, out_re, out_im, ok):
        cs_ = slice(512 * j, 512 * (j + 1))
        tt('dve', ta[0], re_ps, Tre[:, cs_], ALU.mult, [rk, Tk], ['ta0'])
        tt('dve', ta[1], im_ps, Tim[:, cs_], ALU.mult, [ik, Tk], ['ta1'])
        tt('pool', out_re, ta[0], ta[1], ALU.subtract, ['ta0', 'ta1'], [ok])
        tt('dve', ta[2], im_ps, Tre[:, cs_], ALU.mult, [ik, Tk], ['ta2'])
        tt('dve', ta[3], re_ps, Tim[:, cs_], ALU.mult, [rk, Tk], ['ta3'])
        tt('pool', out_im, ta[2], ta[3], ALU.add, ['ta2', 'ta3'], [ok])

    for t in range(NT if L.stop != 's5a' else 0):
        tok = slice(t * 128, (t + 1) * 128)
        u, uk = uT.next()
        S.dma('sp', u, L.u5T_s[:, :, tok].rearrange("c p t -> p c t"), ['u5T_s%d' % t], [uk])
        for part in range(2):
            for c in range(2):
                b = part * 2 + c
                mm(PB[b][:, 0:512], u[:, c, :], Bblk[:, b, :], True, True, [uk, 'Bblk'], [PBk[b]])
        for j in range(2):
            cmul(PB[j][:, 0:512], PB[2 + j][:, 0:512], PBk[j], PBk[2 + j], T1re, T1im, 'T1', j,
                 V[:, 512 * j:512 * (j + 1)], V[:, 1024 + 512 * j:1024 + 512 * (j + 1)], 'V')
        x, xk = X.next()
        xp, xpk = X.prev()
        for b in range(4):
            mm(PB[4 + b][:, 0:512], U_b, V[:, 512 * b:512 * (b + 1)], True, t == 0, ['V', 'U_b'], [PBk[4 + b]])
            if t > 0:
                mm(PB[4 + b][:, 0:512], C('Sel'), xp[:, 512 * b:512 * (b + 1)], False, True, [xpk, 'cst'], [PBk[4 + b]])
        for j in range(2):
            cmul(PB[4 + j][:, 0:512], PB[6 + j][:, 0:512], PBk[4 + j], PBk[6 + j], T2re, T2im, 'T2', j,
                 x[:, 512 * j:512 * (j + 1)], x[:, 1024 + 512 * j:1024 + 512 * (j + 1)], xk)
        cp('act', Xb, x, [xk], ['Xb'])
        for blk in range(16):
            pv = bview(blk // 8)
            tr(pv[:, (blk % 8) * 128:(blk % 8 + 1) * 128], Xb[:, blk * 128:(blk + 1) * 128], ident_b, ['Xb', 'ident_b'], [PBk[blk // 8]])
        for hf in range(2):
            cp('act', XT[:, hf * 8:(hf + 1) * 8, :], bview(hf).rearrange("p (j t) -> p j t", j=8), [PBk[hf]], ['XT'])
        for c in range(2):
            mm(PB[2][:, c * 128:(c + 1) * 128], u[:, c, :], dgd[:, c, :], True, False, [uk, 'dgd'], [PBk[2]])
            for m in range(4 * c, 4 * c + 4):
                for part in range(2):
                    j = m + 8 * part
                    mm(PB[2][:, 32 * m:32 * m + 32], XT[:, j, :], Cblk[:, j, :], False, (m == 4 * c + 3 and part == 1), ['XT', 'Cblk'], [PBk[2]])
        act(y2b, PB[2][:, 0:256], AF.Gelu_apprx_tanh, [PBk[2]], ['y2b'])
        pv = PB[3].bitcast(BF16)[:, 0:256]
        for c in range(2):
            tr(pv[:, c * 128:(c + 1) * 128], y2b[:, c * 128:(c + 1) * 128], ident_b, ['y2b', 'ident_b'], [PBk[3]])
        cp('act', y2T, pv.rearrange("p (c t) -> p c t", c=2), [PBk[3]], ['y2T'])
        for co in range(2):
            for c in range(2):
                mm(PB[3][:, 128 + 128 * co:256 + 128 * co], glb[:, c, co * 128:(co + 1) * 128], y2T[:, c, :], c == 0, c == 1,
                   ['glb', 'y2T'], [PBk[3]])
        for co in range(2):
            act(sgm[:, co, :], PB[3][:, 128 + 128 * co:256 + 128 * co], AF.Sigmoid, [PBk[3], 'dg2'], ['sgm'], bias=dg2[:, 2 + co:3 + co])
        so, sok = s5o.next()
        tt('dve', so, y2T, sgm, ALU.mult, ['y2T', 'sgm'], [sok])
        S.dma('pool', L.catT_s[4:6, :, tok].rearrange("c p t -> p c t"), so, [sok], ['catT_s5_%d' % t])


def phase_ssd(S, Ld):
    L = _NS(Ld)
    A, mm, tr, act, cp, tt, ts, stt, memset, bview = L.A, L.mm, L.tr, L.act, L.cp, L.tt, L.ts, L.stt, L.memset, L.bview
    PB, PBk, C, l, NT = L.PB, L.PBk, L.C, L.l, L.NT
    ident_b = L.ident_b
    L.new_phase()
    pps = A([128, 16])
    o0 = PP['dtb'][0]
    S.dma('sp', pps[:, 0:14], L.pp_d[l, :, o0:o0 + 14], ['pp_d'], ['pps'])
    dsk = pps[:, 8:12]
    xsb = Rot([A([128, 384], BF16), A([128, 384], BF16)], 'xsb')
    bcT = Rot([A([128, 2, 128], BF16), A([128, 2, 128], BF16)], 'bcT')
    zs = Rot([A([128, 256]), A([128, 256])], 'zs')
    dts = Rot([A([128, 32]), A([128, 32])], 'dts')
    rhs4 = A([128, 4, 128])
    E4 = A([128, 4, 128])
    acs8 = A([128, 8])
    ea = A([128, 4]); es = A([128, 4]); et = A([128, 4]); wdt = A([128, 4])
    mtmp = A([128, 128])
    GTs = A([128, 256])
    stS = A([128, 128])
    MTb = A([128, 4, 128], BF16)
    Bw2 = A([128, 4, 128], BF16)
    memset('pool', Bw2, 0.0, ['Bw2'])
    prevT = A([128, 2, 64])
    prevTb = A([128, 2, 64], BF16)
    memset('pool', prevT, 0.0, ['prevT'])
    memset('pool', prevTb, 0.0, ['prevTb'])
    yt = A([128, 256]); y = A([128, 256]); sz = A([128, 256]); junk = A([128, 128])
    ssg = A([128, 2])
    yb = A([128, 256], BF16)
    so = Rot([A([128, 2, 128], BF16), A([128, 2, 128], BF16)], 'sso')
    for t in range(NT):
        tok = slice(t * 128, (t + 1) * 128)
        xs_, xsk = xsb.next()
        S.dma('sp', xs_, L.xsb_s[tok, :], ['xsb_s%d' % t], [xsk])
        bc, bck = bcT.next()
        S.dma('pool', bc, L.bcT_s[:, :, tok].rearrange("c p t -> p c t"), ['bcT_s%d' % t], [bck])
        z_, zk = zs.next()
        S.dma('sp', z_, L.zs_s[tok, :], ['zs_s%d' % t], [zk])
        d_, dk = dts.next()
        S.dma('pool', d_, L.dt_s[tok, :], ['dt_s%d' % t], [dk])
        dt_ = d_[:, 0:4]
        adt = d_[:, 16:20]
        for h in range(4):
            ts('dve', rhs4[:, h, :], C('U'), adt[:, h:h + 1], None, ALU.mult, None, ['cst', dk], ['rhs4'])
        mm(PB[0][:, 0:512], ident_b, L.NEGsl4_b, True, False, ['ident_b', 'NEGsl4_b'], [PBk[0]])
        for h in range(4):
            mm(PB[0][:, 128 * h:128 * (h + 1)], C('M1'), rhs4[:, h, :], False, h == 3, ['cst', 'rhs4'], [PBk[0]])
        act(E4.rearrange("p h l -> p (h l)"), PB[0][:, 0:512], AF.Exp, [PBk[0]], ['E4'])
        if L.stop == 'ssd1':
            continue
        mm(PB[1][:, 0:4], C('U'), adt, True, True, ['cst', dk], [PBk[1]])
        mm(PB[1][:, 4:8], C('ones'), adt, True, True, ['cst', dk], [PBk[1]])
        cp('act', acs8, PB[1][:, 0:8], [PBk[1]], ['acs8'])
        act(ea, acs8[:, 0:4], AF.Exp, ['acs8'], ['ea'])
        act(et, acs8[:, 4:8], AF.Exp, ['acs8'], ['et'])
        tt('dve', es, acs8[:, 4:8], acs8[:, 0:4], ALU.subtract, ['acs8'], ['es'])
        act(es, es, AF.Exp, ['es'], ['es'])
        tt('dve', wdt, es, dt_, ALU.mult, ['es', dk], ['wdt'])
        if L.stop == 'ssd2':
            continue
        if L.stop != 'ssd2c':
            for g in range(2):
                mm(PB[2][:, 128 * g:128 * (g + 1)], bc[64 * g:64 * g + 64, 0, :], bc[64 * g:64 * g + 64, 1, :], True, True, [bck], [PBk[2]], selfsync=True)
            cp('act', GTs, PB[2][:, 0:256], [PBk[2]], ['GTs'])
        else:
            memset('pool', GTs, 1.0, ['GTs'])
        if L.stop == 'ssd2b':
            continue
        for h in range(4):
            g = h // 2
            stt('dve', mtmp, GTs[:, 128 * g:128 * (g + 1)], dt_[:, h:h + 1], E4[:, h, :], ALU.mult, ALU.mult, ['GTs', dk, 'E4'], ['mtmp'])
            stt('dve', MTb[:, h, :], C('ident'), dsk[:, h:h + 1], mtmp, ALU.mult, ALU.add, ['cst', 'pps', 'mtmp'], ['MTb'])
        if L.stop == 'ssd3':
            continue
        for h in range(4):
            mm(PB[3][:, 64 * h:64 * (h + 1)], MTb[:, h, :], xs_[:, 64 * h:64 * (h + 1)], True, True, ['MTb', xsk], [PBk[3]])
        for h in range(4):
            g, r = h // 2, h % 2
            mm(PB[4][:, 64 * h:64 * (h + 1)], bc[64 * g:64 * g + 64, 1, :], prevTb[64 * g:64 * g + 64, r, :], True, True, [bck, 'prevTb'], [PBk[4]], selfsync=True)
        tt('dve', yt.rearrange("p (h q) -> p h q", h=4), PB[4][:, 0:256].rearrange("p (h q) -> p h q", h=4),
           ea.unsqueeze(2).to_broadcast([128, 4, 64]), ALU.mult, [PBk[4], 'ea'], ['yt'])
        tt('dve', y, yt, PB[3][:, 0:256], ALU.add, ['yt', PBk[3]], ['y'])
        if L.stop == 'ssd4':
            continue
        for h in range(4):
            g = h // 2
            ts('pool', Bw2[:, h, 64 * g:64 * g + 64], xs_[:, 256 + 64 * g:256 + 64 * g + 64], wdt[:, h:h + 1], None, ALU.mult, None,
               [xsk, 'wdt'], ['Bw2'])
        for r in range(2):
            for g in range(2):
                h = 2 * g + r
                mm(PB[5][:, 64 * r:64 * (r + 1)], Bw2[:, h, :], xs_[:, 64 * h:64 * (h + 1)], g == 0, g == 1, ['Bw2', xsk], [PBk[5]])
        cp('act', stS, PB[5][:, 0:128], [PBk[5]], ['stS'])
        for r in range(2):
            for g in range(2):
                h = 2 * g + r
                ps_ = slice(64 * g, 64 * g + 64)
                stt('dve', prevT[ps_, r, :], prevT[ps_, r, :], et[ps_, h:h + 1], stS[ps_, 64 * r:64 * (r + 1)], ALU.mult, ALU.add,
                    ['prevT', 'et', 'stS'], ['prevT'])
        cp('pool', prevTb, prevT, ['prevT'], ['prevTb'])
        if L.stop == 'ssd5':
            continue
        act(sz, z_, AF.Silu, [zk], ['sz'])
        tt('dve', y, y, sz, ALU.mult, ['y', 'sz'], ['y'])
        memset('pool', ssg, 0.0, ['ssg'])
        for g in range(2):
            act(junk, y[:, 128 * g:128 * (g + 1)], AF.Square, ['y'], ['junkS', 'ssg'], accum_out=ssg[:, g:g + 1])
        L.rstd_inplace(ssg, 'ssg', 1.0 / 128)
        tt('dve', yb.rearrange("p (g q) -> p g q", g=2), y.rearrange("p (g q) -> p g q", g=2),
           ssg.unsqueeze(2).to_broadcast([128, 2, 128]), ALU.mult, ['y', 'ssg'], ['yb'])
        pv = PB[6].bitcast(BF16)[:, 0:256]
        for c in range(2):
            tr(pv[:, c * 128:(c + 1) * 128], yb[:, c * 128:(c + 1) * 128], ident_b, ['yb', 'ident_b'], [PBk[6]])
        o_, ok_ = so.next()
        cp('act', o_, pv.rearrange("p (c t) -> p c t", c=2), [PBk[6]], [ok_])
        S.dma('sp', L.catT_s[6:8, :, tok].rearrange("c p t -> p c t"), o_, [ok_], ['catT_ssd_%d' % t])


def phase_attn(S, Ld):
    L = _NS(Ld)
    A, mm, tr, act, cp, tt, ts, stt, memset, bview = L.A, L.mm, L.tr, L.act, L.cp, L.tt, L.ts, L.stt, L.memset, L.bview
    PB, PBk, C, l, NT, SQ, NQB, TOPK = L.PB, L.PBk, L.C, L.l, L.NT, L.SQ, L.NQB, L.TOPK
    ident_b = L.ident_b
    L.new_phase()
    kT2 = A([128, SQ], BF16)
    kiT2 = A([128, SQ], BF16)
    Vt = A([128, NT, 130], BF16)
    S.dma('sp', kT2, L.kT_s[:, :], ['kT_s%d' % t for t in range(NT)], ['kT2'])
    for hf in range(2):
        S.dma('sp' if hf == 0 else 'pool', kiT2[64 * hf:64 * hf + 64, :], L.kiT_s[:, :], ['kiT_s%d' % t for t in range(NT)], ['kiT2'])
    v3 = L.v_s.rearrange("(t p) c -> p t c", p=128)
    for t0 in range(0, NT, 8):
        t1 = min(NT, t0 + 8)
        S.dma('sp' if (t0 // 8) % 2 == 0 else 'pool', Vt[:, t0:t1, :], v3[:, t0:t1, :], ['v_s%d' % t for t in range(t0, t1)], ['Vt'])
    qT = A([128, 4, 512], BF16)
    qiT = A([128, 4, 512], BF16)
    sgn = A([128, 4, 8])
    dg = Rot([A([128, 8, 128], BF16), A([128, 8, 128], BF16)], 'dg')
    Rr = Rot([A([128, 512], BF16) for _ in range(3)], 'R')
    sc = A([128, SQ])
    NKB = NT
    maskT = A([128, NKB, 512], BF16)
    mrow = A([128, SQ], BF16)
    lo = A([128, 1]); w0 = A([128, 1]); mid = A([128, 1]); cnt = A([128, 1]); inc = A([128, 1]); hi = A([128, 1])
    wtab = A([128, NIT])
    Er = Rot([A([128, 512], BF16) for _ in range(3)], 'E')
    rden = A([128, 512])
    bcs = A([64, 512])
    Ob = Rot([A([64, 512], BF16), A([64, 512], BF16)], 'Ob')
    psr = Rot([PB[0], PB[1], PB[2], PB[3]], PBk[0:4])
    pacc = Rot([PB[4], PB[5]], PBk[4:6])
    for Q in range(NQB):
        qs = slice(Q * 512, (Q + 1) * 512)
        S.dma('sp', qT, L.qT_s[:, :, qs].rearrange("j p t -> p j t"), ['qT_s%d' % t for t in range(4 * Q, 4 * Q + 4)], ['qT'])
        S.dma('pool', qiT, L.qiT_s[:, :, qs].rearrange("j p t -> p j t"), ['qiT_s%d' % t for t in range(4 * Q, 4 * Q + 4)], ['qiT'])
        S.dma('sp', sgn, L.sg_s[qs, :].rearrange("(i p) h -> p i h", p=128), ['sg_s%d' % t for t in range(4 * Q, 4 * Q + 4)], ['sgn'])
        memset('pool', maskT[:, 4 * Q:4 * Q + 4, :], -30000.0, ['maskT'])
        for i in range(4):
            qt = 4 * Q + i
            Lk = 128 * (qt + 1)
            d_, dgk = dg.next()
            for h in range(8):
                ts('pool', d_[:, h, :], ident_b, sgn[:, i, h:h + 1], None, ALU.mult, None, ['ident_b', 'sgn'], [dgk])
            nkc = (Lk + 511) // 512
            for kc in range(nkc):
                wd = min(512, Lk - 512 * kc)
                ks = slice(512 * kc, 512 * kc + wd)
                pa, pak = pacc.next()
                for h in range(8):
                    pr, prk = psr.next()
                    hp = slice(64 * (h % 2), 64 * (h % 2) + 64)
                    mm(pr[:, 0:wd], qiT[hp, h // 2, 128 * i:128 * (i + 1)], kiT2[hp, ks], True, True, ['qiT', 'kiT2'], [prk])
                    R, Rk = Rr.next()
                    act(R[:, 0:wd], pr[:, 0:wd], AF.Relu, [prk], [Rk])
                    mm(pa[:, 0:wd], d_[:, h, :], R[:, 0:wd], h == 0, h == 7, [dgk, Rk], [pak])
                cp('dve', sc[:, ks], pa[:, 0:wd], [pak], ['sc'])
            tt('dve', sc[:, Lk - 128:Lk], sc[:, Lk - 128:Lk], C('NEGq'), ALU.add, ['sc', 'cst'], ['sc'])
            if Lk > TOPK:
                S.op('dve', lambda e, Lk=Lk: e.tensor_reduce(lo, sc[:, 0:Lk - 128], AX.X, ALU.min), ['sc'], ['lo'])
                S.op('dve', lambda e, Lk=Lk: e.tensor_reduce(hi, sc[:, 0:Lk], AX.X, ALU.max), ['sc'], ['hi'])
                tt('dve', w0, hi, lo, ALU.subtract, ['hi', 'lo'], ['w0'])
                ts('dve', wtab, C('pow2'), w0, None, ALU.mult, None, ['cst', 'w0'], ['wtab'])
                for k in range(NIT):
                    tt('dve', mid, lo, wtab[:, k:k + 1], ALU.add, ['lo', 'wtab'], ['mid'])
                    memset('dve', cnt, 0.0, ['cnt'])
                    ts('dve', mrow[:, 0:Lk], sc[:, 0:Lk], mid, 0.0, ALU.is_ge, ALU.add, ['sc', 'mid'], ['mrow', 'cnt'], accum_out=cnt)
                    stt('dve', inc, cnt, TOPK - 0.5, wtab[:, k:k + 1], ALU.is_ge, ALU.mult, ['cnt', 'wtab'], ['inc'])
                    tt('dve', lo, lo, inc, ALU.add, ['lo', 'inc'], ['lo'])
            else:
                memset('dve', lo, -1.0e38, ['lo'])
            if L.debug and Lk > TOPK:
                dbt = L.A([128, 4])
                cp('dve', dbt[:, 0:1], lo, ['lo'], ['dbt'])
                cp('dve', dbt[:, 1:2], hi, ['hi'], ['dbt'])
                cp('dve', dbt[:, 2:3], cnt, ['cnt'], ['dbt'])
                cp('dve', dbt[:, 3:4], w0, ['w0'], ['dbt'])
                S.dma('sp', L.tk_s[128 * qt:128 * (qt + 1), :], dbt, ['dbt'], ['tk_s%d' % qt])
            ts('dve', mrow[:, 0:Lk], sc[:, 0:Lk], lo, None, ALU.is_ge, None, ['sc', 'lo'], ['mrow'])
            nkb = Lk // 128
            for b0 in range(0, nkb, 8):
                nb = min(8, nkb - b0)
                pt, ptk = psr.next()
                pv = pt.bitcast(BF16)
                for j in range(nb):
                    tr(pv[:, 128 * j:128 * (j + 1)], mrow[:, 128 * (b0 + j):128 * (b0 + j + 1)], ident_b, ['mrow', 'ident_b'], [ptk])
                ts('dve', maskT[:, b0:b0 + nb, 128 * i:128 * (i + 1)], pv[:, 0:128 * nb].rearrange("p (j t) -> p j t", j=nb),
                   30000.0, -30000.0, ALU.mult, ALU.add, [ptk], ['maskT'])
        nkb = 4 * Q + 4
        for h in range(8):
            g = h // 4
            hp = slice(64 * g, 64 * g + 64)
            pa, pak = pacc.next()
            for kb in range(nkb):
                pr, prk = psr.next()
                mm(pr[:, 0:512], kT2[hp, 128 * kb:128 * (kb + 1)], qT[hp, h % 4, :], True, False, ['kT2', 'qT'], [prk])
                mm(pr[:, 0:512], ident_b, maskT[:, kb, :], False, True, ['ident_b', 'maskT'], [prk])
                E, Ek = Er.next()
                act(E, pr[:, 0:512], AF.Exp, [prk], [Ek], scale=0.125)
                mm(pa[0:65, 0:512], Vt[:, kb, 65 * g:65 * g + 65], E, kb == 0, kb == nkb - 1, ['Vt', Ek], [pak])
            S.op('dve', lambda e, pa=pa: e.reciprocal(rden[64:65, :], pa[64:65, 0:512]), [pak], ['rden'])
            pr, prk = psr.next()
            mm(pr[0:64, 0:512], C('ones')[64:65, 0:64], rden[64:65, :], True, True, ['cst', 'rden'], [prk])
            cp('act', bcs, pr[0:64, 0:512], [prk], ['bcs'])
            o_, ok_ = Ob.next()
            tt('dve', o_, pa[0:64, 0:512], bcs, ALU.mult, [pak, 'bcs'], [ok_])
            S.dma('sp', L.catT_s[h // 2, 64 * (h % 2):64 * (h % 2) + 64, qs], o_, [ok_], ['catT_at%d_%d' % (Q, h)])


def phase_ffn(S, Ld):
    L = _NS(Ld)
    A, mm, tr, act, cp, tt, ts, stt, memset, bview = L.A, L.mm, L.tr, L.act, L.cp, L.tt, L.ts, L.stt, L.memset, L.bview
    PB, PBk, C, l, NT, SQ, NQB = L.PB, L.PBk, L.C, L.l, L.NT, L.SQ, L.NQB
    ident_b = L.ident_b
    L.new_phase()
    ppf = A([128, 32])
    S.dma('sp', ppf[:, 0:16], L.pp_d[l, :, 0:16], ['pp_d'], ['ppf'])
    gs = A([128, 2])
    o0 = PP['gssd'][0]
    S.dma('sp', gs, L.pp_d[l, :, o0:o0 + 2], ['pp_d'], ['gs'])
    Wout_b = A([128, 8, 1024], BF16)
    Wup_b = A([128, 8, 4096], BF16)
    stg = Rot([A([128, 2048]), A([128, 2048])], 'stgf')
    n_ = 0
    for c in range(8):
        for hf in range(1):
            s_, sk = stg.next()
            S.dma('sp' if n_ % 2 == 0 else 'pool', s_[:, 0:1024], L.wout_d[l, c * 128:(c + 1) * 128, :], ['wout_d'], [sk])
            n_ += 1
            if c >= 6:
                ts('dve', Wout_b[:, c, :], s_[:, 0:1024], gs[:, c - 6:c - 5], None, ALU.mult, None, [sk, 'gs'], ['Wout_b'])
            else:
                cp('dve', Wout_b[:, c, :], s_[:, 0:1024], [sk], ['Wout_b'])
    for c in range(8):
        for hf in range(2):
            s_, sk = stg.next()
            S.dma('sp' if n_ % 2 == 0 else 'pool', s_, L.wup_d[l, c * 128:(c + 1) * 128, hf * 2048:(hf + 1) * 2048], ['wup_d'], [sk])
            n_ += 1
            ts('dve' if hf == 0 else 'pool', Wup_b[:, c, hf * 2048:(hf + 1) * 2048], s_, ppf[:, 8 + c:9 + c], None, ALU.mult, None,
               [sk, 'ppf'], ['Wup_b'])
    xrot = Rot([A([128, 1024]), A([128, 1024])], 'fx')
    crot = Rot([A([128, 8, 128], BF16), A([128, 8, 128], BF16)], 'fc')
    x1r = Rot([A([128, 1024]), A([128, 1024])], 'x1')
    h2b = A([128, 1024], BF16)
    h2T = A([128, 8, 512], BF16)
    junk = A([128, 1024]); ss = A([128, 1])
    rl = Rot([A([128, 512]), A([128, 512])], 'rl')
    aTr = Rot([A([128, 4, 512], BF16), A([128, 4, 512], BF16)], 'aT')
    pup = Rot([PB[2], PB[3], PB[4], PB[5]], PBk[2:6])
    for Q in range(NQB):
        for j in range(4):
            t = 4 * Q + j
            tok = slice(t * 128, (t + 1) * 128)
            xt, xk = xrot.next()
            S.dma('sp', xt, L.xsrc[tok, :], ['xin%d' % t], [xk])
            ct, ck = crot.next()
            rk = ['catT_s5_%d' % t, 'catT_ssd_%d' % t] + ['catT_at%d_%d' % (Q, h) for h in range(8)]
            S.dma('pool', ct, L.catT_s[:, :, tok].rearrange("c p t -> p c t"), rk, [ck])
            for n in range(2):
                for c in range(8):
                    mm(PB[n][:, 0:512], ct[:, c, :], Wout_b[:, c, n * 512:(n + 1) * 512], c == 0, c == 7, [ck, 'Wout_b'], [PBk[n]])
            x1, x1k = x1r.next()
            for n in range(2):
                tt('dve', x1[:, n * 512:(n + 1) * 512], xt[:, n * 512:(n + 1) * 512], PB[n][:, 0:512], ALU.add, [xk, PBk[n]], [x1k])
            S.dma('sp', L.xb_d[tok, :], x1, [x1k], ['xin%d' % t])
            memset('pool', ss, 0.0, ['ss'])
            act(junk, x1, AF.Square, [x1k], ['junkF', 'ss'], accum_out=ss)
            L.rstd_inplace(ss, 'ss', 1.0 / 1024)
            ts('dve', h2b, x1, ss, None, ALU.mult, None, [x1k, 'ss'], ['h2b'])
            pT = bview(7)
            for c in range(8):
                tr(pT[:, c * 128:(c + 1) * 128], h2b[:, c * 128:(c + 1) * 128], ident_b, ['h2b', 'ident_b'], [PBk[7]])
            cp('act', h2T[:, :, j * 128:(j + 1) * 128], pT.rearrange("p (c t) -> p c t", c=8), [PBk[7]], ['h2T'])
        for f in range(32):
            pu, puk = pup.next()
            for c in range(8):
                mm(pu[:, 0:512], Wup_b[:, c, f * 128:(f + 1) * 128], h2T[:, c, :], c == 0, c == 7, ['Wup_b', 'h2T'], [puk])
            r_, rk_ = rl.next()
            act(r_, pu[:, 0:512], AF.Relu, [puk], [rk_])
            if f % 4 == 0:
                aT, aTk = aTr.next()
            tt('pool' if f % 2 == 0 else 'dve', aT[:, f % 4, :], r_, r_, ALU.mult, [rk_], [aTk])
            if f % 4 == 3:
                S.dma('sp' if (f // 4) % 2 == 0 else 'pool', L.aT_s[f - 3:f + 1, :, Q * 512:(Q + 1) * 512].rearrange("f p t -> p f t"), aT,
                      [aTk], ['aT_s%d_%d' % (Q, f // 4)])
    L.new_phase()
    Wdn_b = A([128, 32, 1024], BF16)
    stg = Rot([A([128, 2048]), A([128, 2048])], 'stgd')
    for f in range(0, 32, 2):
        s_, sk = stg.next()
        S.dma('sp' if (f // 2) % 2 == 0 else 'pool', s_.rearrange("p (a n) -> p a n", a=2),
              L.wdn_d[l, f * 128:(f + 2) * 128, :].rearrange("(a p) n -> p a n", p=128), ['wdn_d'], [sk])
        cp('dve' if (f // 2) % 2 == 0 else 'pool', Wdn_b[:, f:f + 2, :].rearrange("p a n -> p (a n)"), s_, [sk], ['Wdn_b'])
    aTl = Rot([A([128, 32, 512], BF16), A([128, 32, 512], BF16)], 'aTl')
    xrot = Rot([A([128, 1024]), A([128, 1024])], 'dx')
    orot = Rot([A([128, 1024]), A([128, 1024])], 'do')
    pd = Rot([PB[0], PB[1], PB[2], PB[3]], PBk[0:4])
    for Q in range(NQB):
        a_, ak = aTl.next()
        for f4 in range(8):
            S.dma('sp' if f4 % 2 == 0 else 'pool', a_[:, 4 * f4:4 * f4 + 4, :],
                  L.aT_s[4 * f4:4 * f4 + 4, :, Q * 512:(Q + 1) * 512].rearrange("f p t -> p f t"), ['aT_s%d_%d' % (Q, f4)], [ak])
        for j in range(4):
            t = 4 * Q + j
            tok = slice(t * 128, (t + 1) * 128)
            xt, xk = xrot.next()
            S.dma('sp', xt, L.xb_d[tok, :], ['xin%d' % t], [xk])
            ot, ok_ = orot.next()
            for n in range(2):
                p_, pk = pd.next()
                for f in range(32):
                    mm(p_[:, 0:512], a_[:, f, j * 128:(j + 1) * 128], Wdn_b[:, f, n * 512:(n + 1) * 512], f == 0, f == 31, [ak, 'Wdn_b'], [pk])
                tt('dve', ot[:, n * 512:(n + 1) * 512], xt[:, n * 512:(n + 1) * 512], p_[:, 0:512], ALU.add, [xk, pk], [ok_])
            S.dma('pool', L.xdst[tok, :], ot, [ok_], ['xin%d' % t])


def make_in_maps(inp, SQ, NL, nb):
    cst, cs = host_consts(SQ)
    common = {
        'w_in': np.ascontiguousarray(np.asarray(inp['w_in'], np.float32)[:NL][:, :, W_IN_PERM]),
        'w_out': np.ascontiguousarray(np.asarray(inp['w_out'], np.float32)[:NL]),
        'w_up': np.ascontiguousarray(np.asarray(inp['w_up'], np.float32)[:NL]),
        'w_down': np.ascontiguousarray(np.asarray(inp['w_down'], np.float32)[:NL]),
        'glu_w': np.ascontiguousarray(np.asarray(inp['s5_glu_w'], np.float32)[:NL]),
        'pp': np.stack([host_pp(inp, l) for l in range(NL)]),
        'cst': cst, 'cs': cs,
    }
    return [dict(common, x=np.ascontiguousarray(np.asarray(inp['x'][b], np.float32))) for b in range(nb)]


_NC_CACHE = {}
FUSED = False


def _layer_slice(inp, l):
    out = {}
    for k, v in inp.items():
        out[k] = v if k == 'x' else np.asarray(v)[l:l + 1]
    return out


def kernel(**inputs):
    inp = {k: np.asarray(v) for k, v in inputs.items()}
    B, SQ, _ = inp['x'].shape
    NL = inp['w_in'].shape[0]
    if FUSED:
        key = (SQ, NL)
        if key not in _NC_CACHE:
            _NC_CACHE[key] = build(SQ, NL)
        in_maps = make_in_maps(inp, SQ, NL, B)
        res = run_bass_kernel_spmd(_NC_CACHE[key], in_maps, core_ids=list(range(B)))
        return np.stack([np.asarray(r['out'], np.float32) for r in res.results]).astype(np.float32)
    key = (SQ, 1)
    if key not in _NC_CACHE:
        _NC_CACHE[key] = build(SQ, 1)
    x = np.asarray(inp['x'], np.float32)
    for l in range(NL):
        li = _layer_slice(inp, l)
        li['x'] = x
        in_maps = make_in_maps(li, SQ, 1, B)
        res = run_bass_kernel_spmd(_NC_CACHE[key], in_maps, core_ids=list(range(B)))
        x = np.stack([np.asarray(r['out'], np.float32) for r in res.results]).astype(np.float32)
    return x
```

```python
import numpy as np
import sys
from contextlib import ExitStack
import concourse.bass as bass
import concourse.mybir as mybir
from concourse.bass_utils import run_bass_kernel_spmd

F32 = mybir.dt.float32
BF16 = mybir.dt.bfloat16
AF = mybir.ActivationFunctionType
ALU = mybir.AluOpType
AX = mybir.AxisListType


class Sched:
    ENG = ('pe', 'act', 'dve', 'pool', 'sp')
    NDQ = 6

    def __init__(self, nc):
        self.nc = nc
        self.prog = {e: [] for e in self.ENG}
        self.cnt = {e: 0 for e in self.ENG}
        self.waited = {e: {} for e in self.ENG}
        self.lw = {}
        self.rd = {}
        self.dma_i = {e: 0 for e in self.ENG}
        self.dma_cnt = {}
        self.sems = {}
        self.stack = ExitStack()
        self.n_ops = 0

    def ctx(self):
        nc = self.nc
        for e in ('pe', 'act', 'dve', 'pool'):
            self.sems[e] = self.stack.enter_context(nc.semaphore('s_' + e))
        for q in ('sp', 'pool', 'act'):
            for j in range(self.NDQ):
                k = 'd%s%d' % (q, j)
                self.sems[k] = self.stack.enter_context(nc.semaphore('s_' + k))
                self.dma_cnt[k] = 0
        return self.stack

    def _nm(self, p):
        self.n_names = getattr(self, 'n_names', 0) + 1
        return '%s%d' % (p, self.n_names)

    def sb(self, shape, dtype, name=None):
        return self.stack.enter_context(self.nc.sbuf_tensor(self._nm('sb'), list(shape), dtype))

    def psum(self):
        return self.stack.enter_context(self.nc.psum_tensor(self._nm('ps'), [128, 512], F32))

    def psum_bf16(self):
        return self.stack.enter_context(self.nc.psum_tensor(self._nm('pb'), [128, 1024], BF16))

    def _deps(self, eng, reads, writes):
        deps = {}

        def add(tok):
            if tok is None:
                return
            k, v = tok
            if deps.get(k, 0) < v:
                deps[k] = v
        for b in reads:
            add(self.lw.get(b))
            if isinstance(b, str) and b.startswith('pb'):
                for k, v in self.rd.get(b, {}).items():
                    if k != eng:
                        add((k, v))
        for b in writes:
            add(self.lw.get(b))
            for k, v in self.rd.get(b, {}).items():
                add((k, v))
        waits = []
        w = self.waited[eng]
        for k, v in deps.items():
            if eng == 'pe' and k == 'pe':
                continue
            if w.get(k, 0) >= v:
                continue
            w[k] = v
            waits.append((k, v))
        return waits

    def _commit(self, tok, reads, writes):
        for b in writes:
            self.lw[b] = tok
            self.rd[b] = {}
        for b in reads:
            if b in writes:
                continue
            d = self.rd.setdefault(b, {})
            if d.get(tok[0], 0) < tok[1]:
                d[tok[0]] = tok[1]

    def op(self, eng, fn, reads=(), writes=(), selfsync=False):
        waits = self._deps(eng, reads, writes)
        if selfsync and self.cnt[eng] > 0 and self.waited[eng].get(eng, 0) < self.cnt[eng]:
            self.waited[eng][eng] = self.cnt[eng]
            waits.append((eng, self.cnt[eng]))
        self.cnt[eng] += 1
        tok = (eng, self.cnt[eng])
        self.prog[eng].append((waits, fn, (eng, 1), self._where()))
        self._commit(tok, reads, writes)
        self.n_ops += 1
        return tok

    def dma(self, q, out, in_, reads=(), writes=(), final=False, **kw):
        waits = self._deps(q, reads, writes)
        j = self.dma_i[q] % self.NDQ
        self.dma_i[q] += 1
        k = 'd%s%d' % (q, j)
        prev = 16 * self.dma_cnt[k]
        if prev > 0 and self.waited[q].get(k, 0) < prev:
            self.waited[q][k] = prev
            waits.append((k, prev))
        self.dma_cnt[k] += 1
        tok = (k, 16 * self.dma_cnt[k])
        self.prog[q].append((waits, lambda e: e.dma_start(out=out, in_=in_, **kw), (k, 16), self._where()))
        self._commit(tok, reads, writes)
        self.n_ops += 1
        return tok

    def _where(self):
        out = []
        f = sys._getframe(2)
        while f is not None and len(out) < 4:
            out.append(f.f_lineno)
            f = f.f_back
        return out

    def barrier(self):
        toks = {}
        for e in ('pe', 'act', 'dve', 'pool'):
            if self.cnt[e] > 0:
                toks[e] = self.cnt[e]
        for k, c in self.dma_cnt.items():
            if c > 0:
                toks[k] = 16 * c
        for e in self.ENG:
            w = self.waited[e]
            waits = []
            for k, v in toks.items():
                if k == e:
                    continue
                if w.get(k, 0) < v:
                    w[k] = v
                    waits.append((k, v))
            if waits:
                self.prog[e].append((waits, None, None, None))

    def _emit(self, eng, e):
        sems = self.sems
        for waits, fn, inc, where in self.prog[eng]:
            for k, v in waits:
                e.wait_ge(sems[k], v)
            if fn is not None:
                try:
                    ins = fn(e)
                except Exception:
                    print("EMIT ERROR at lines", where, file=sys.stderr)
                    raise
                ins.then_inc(sems[inc[0]], inc[1])

    def finish(self):
        self.barrier()
        nc = self.nc
        with nc.Block() as block:
            @block.sync
            def _(e):
                self._emit('sp', e)

            @block.tensor
            def _(e):
                self._emit('pe', e)

            @block.scalar
            def _(e):
                self._emit('act', e)

            @block.vector
            def _(e):
                self._emit('dve', e)

            @block.gpsimd
            def _(e):
                self._emit('pool', e)


D = 1024
DFF = 4096
EPS = 1e-6
NIT = 20
IDX_W_SCALE = (8 ** -0.5) * (64 ** -0.5)
TWO_PI = 6.283185307179586
NEG_BIG = -3.0e38


def _layout(items):
    off = {}
    o = 0
    for n, w in items:
        off[n] = (o, w)
        o += w
    return off, o


PP_ITEMS = [('gmix', 8), ('gmlp', 8), ('gq', 512), ('gk', 128), ('gki', 64), ('dtb', 4), ('alog', 4),
            ('dskip', 4), ('gssd', 2), ('convw', 2048), ('convb_row', 512), ('convb_col', 2),
            ('lr_row', 1024), ('li_row', 1024), ('ls_row', 1024),
            ('lrB', 128), ('liB', 128), ('lsB', 128), ('bre', 128), ('bim', 128),
            ('cre', 128), ('cim', 128), ('dcol', 2), ('glub', 2)]
PP, NPP = _layout(PP_ITEMS)
CST_ITEMS = [('ident', 128), ('U', 128), ('M1', 128), ('NEGsl4', 512), ('NEGq', 128), ('Sel', 128),
             ('ones', 128), ('n1col', 1), ('pow2', NIT), ('bmask', 512), ('cmask', 32)]
CST, NCST = _layout(CST_ITEMS)
C_Q, C_QI, C_KV, C_Z, C_U5, C_XBC = 0, 512, 1024, 1356, 1612, 1868
CB_Z = 1408


def host_consts(SQ):
    c = np.zeros((128, NCST), np.float32)

    def put(n, a):
        o, w = CST[n]
        c[:, o:o + w] = np.asarray(a, np.float32).reshape(128, w)
    i = np.arange(128)
    put('ident', np.eye(128))
    put('U', (i[:, None] <= i[None, :]))
    put('M1', (i[:, None] > i[None, :]))
    negsl = np.where(i[None, :] < i[:, None], -30000.0, 0.0)
    put('NEGsl4', np.tile(negsl, (1, 4)))
    put('NEGq', np.where(i[None, :] > i[:, None], NEG_BIG, 0.0))
    sel = np.zeros((128, 128))
    sel[127, :] = 1
    put('Sel', sel)
    put('ones', np.ones((128, 128)))
    put('n1col', (i + 1.0)[:, None])
    put('pow2', np.tile(2.0 ** -(np.arange(NIT) + 1.0), (128, 1)))
    bm = np.zeros((128, 8, 64))
    for g in range(8):
        bm[16 * g:16 * g + 16, g, :] = 1
    put('bmask', bm)
    cm = np.zeros((128, 2, 16))
    for g in range(2):
        cm[64 * g:64 * g + 64, g, :] = 1
    put('cmask', cm)
    pos = np.arange(SQ, dtype=np.float32)
    inv_freq = (np.float32(500000.0) ** (-np.arange(0, 16, 2, dtype=np.float32) / np.float32(16))).astype(np.float32)
    ang = (pos[:, None] * inv_freq[None, :]).astype(np.float32)
    cs = np.concatenate([np.cos(ang), np.sin(ang)], axis=1).astype(np.float32)
    return c, cs


_QH = np.concatenate([np.arange(64 * h, 64 * h + 64) for h in (0, 4, 1, 5, 2, 6, 3, 7)])
W_IN_PERM = np.concatenate([_QH, np.arange(768, 1280), np.arange(512, 640), np.arange(640, 768),
                            np.arange(1280, 1344), np.arange(1344, 1352), np.arange(2376, 2380),
                            np.arange(1608, 1864), np.arange(1352, 1608), np.arange(1864, 2376)])


def host_pp(inp, l):
    p = np.zeros((128, NPP), np.float32)

    def put(n, a):
        o, w = PP[n]
        p[:, o:o + w] = np.asarray(a, np.float32).reshape(128, w)

    def rows(v):
        v = np.asarray(v, np.float32).reshape(1, -1)
        return np.tile(v, (128, 1))
    put('gmix', inp['norm_mix_g'][l].reshape(8, 128).T)
    put('gmlp', inp['norm_mlp_g'][l].reshape(8, 128).T)
    put('gq', rows(np.tile(inp['attn_q_norm_g'][l], 8)))
    put('gk', rows(np.tile(inp['attn_k_norm_g'][l], 2)))
    put('gki', rows(inp['idx_k_norm_g'][l]))
    put('dtb', rows(inp['ssd_dt_bias'][l]))
    put('alog', rows(inp['ssd_a_log'][l]))
    put('dskip', rows(inp['ssd_d'][l]))
    put('gssd', inp['ssd_norm_g'][l].reshape(2, 128).T)
    put('convw', rows(inp['ssd_conv_w'][l].reshape(-1)))
    put('convb_row', rows(inp['ssd_conv_b'][l]))
    put('convb_col', inp['ssd_conv_b'][l][256:512].reshape(2, 128).T)
    put('lr_row', rows(inp['s5_lambda_re'][l].reshape(-1)))
    put('li_row', rows(inp['s5_lambda_im'][l].reshape(-1)))
    put('ls_row', rows(np.repeat(inp['s5_log_step'][l], 64)))

    def gh(a):
        a = np.asarray(a, np.float32).reshape(2, 8, 1, 64)
        a = np.broadcast_to(a, (2, 8, 16, 64))
        return a.transpose(1, 2, 0, 3).reshape(128, 128)
    put('lrB', gh(inp['s5_lambda_re'][l]))
    put('liB', gh(inp['s5_lambda_im'][l]))
    put('lsB', gh(np.repeat(inp['s5_log_step'][l][:, None], 64, axis=1)))

    def bl(a):
        a = np.asarray(a, np.float32).reshape(2, 8, 64, 16)
        return a.transpose(1, 3, 0, 2).reshape(128, 128)
    put('bre', bl(inp['s5_b_re'][l]))
    put('bim', bl(inp['s5_b_im'][l]))

    def cl(a):
        a = np.asarray(a, np.float32).reshape(8, 2, 16, 64)
        return a.transpose(1, 3, 0, 2).reshape(128, 128)
    put('cre', cl(inp['s5_c_re'][l]))
    put('cim', cl(inp['s5_c_im'][l]))
    put('dcol', inp['s5_d'][l].reshape(2, 128).T)
    put('glub', inp['s5_glu_b'][l].reshape(2, 128).T)
    return p


class Rot:
    def __init__(self, aps, name):
        self.aps, self.i = aps, 0
        self.keys = list(name) if isinstance(name, (list, tuple)) else ['%s%d' % (name, i) for i in range(len(aps))]

    def next(self):
        i = self.i % len(self.aps)
        self.i += 1
        return self.aps[i], self.keys[i]

    def prev(self):
        i = (self.i - 2) % len(self.aps)
        return self.aps[i], self.keys[i]


def build(SQ, NL, debug=False, stop=None):
    NT = SQ // 128
    NQB = SQ // 512
    TOPK = min(256, SQ // 4)
    nc = bass.Bass("TRN2", target_bir_lowering=False)

    def din(name, shape):
        return nc.dram_tensor(name, list(shape), F32, kind="ExternalInput").ap()

    def dscr(name, shape, dt=BF16):
        return nc.dram_tensor(name, list(shape), dt, kind=("ExternalOutput" if debug else "Internal")).ap()
    x_d = din("x", [SQ, D])
    win_d = din("w_in", [NL, D, 2380])
    wout_d = din("w_out", [NL, D, D])
    wup_d = din("w_up", [NL, D, DFF])
    wdn_d = din("w_down", [NL, DFF, D])
    glu_d = din("glu_w", [NL, 256, 256])
    pp_d = din("pp", [NL, 128, NPP])
    cst_d = din("cst", [128, NCST])
    cs_d = din("cs", [SQ, 16])
    out_d = nc.dram_tensor("out", [SQ, D], F32, kind="ExternalOutput").ap()
    xb_d = nc.dram_tensor("xb", [SQ, D], F32, kind="Internal").ap()
    qT_s = dscr("qT_s", [4, 128, SQ])
    qiT_s = dscr("qiT_s", [4, 128, SQ])
    kT_s = dscr("kT_s", [128, SQ])
    kiT_s = dscr("kiT_s", [64, SQ])
    v_s = dscr("v_s", [SQ, 130])
    sg_s = dscr("sg_s", [SQ, 8], F32)
    u5T_s = dscr("u5T_s", [2, 128, SQ])
    xsb_s = dscr("xsb_s", [SQ, 384])
    bcT_s = dscr("bcT_s", [2, 128, SQ])
    zs_s = dscr("zs_s", [SQ, 256], F32)
    dt_s = dscr("dt_s", [SQ, 32], F32)
    catT_s = dscr("catT_s", [8, 128, SQ])
    aT_s = nc.dram_tensor("aT_s", [32, 128, SQ], BF16, kind="Internal").ap()
    tk_s = dscr("tk_s", [SQ, 4], F32)

    S = Sched(nc)
    with S.ctx():
        ARENA = 52736
        arena = S.sb([128, ARENA], F32)
        PB = [S.psum() for _ in range(8)]
        PBk = ['pb%d' % i for i in range(8)]
        st = {'off': 0}

        def A(shape, dt=F32):
            n = 1
            for s_ in shape[1:]:
                n *= s_
            cols = n if dt == F32 else (n + 1) // 2
            o = st['off']
            st['off'] += cols
            assert st['off'] <= ARENA, "arena overflow %d" % st['off']
            ap = arena[:, o:o + cols]
            if dt != F32:
                ap = ap.bitcast(dt)[:, 0:n]
            if len(shape) == 3:
                ap = ap.rearrange("p (a b) -> p a b", a=shape[1])
            elif len(shape) == 4:
                ap = ap.rearrange("p (a b c) -> p a b c", a=shape[1], b=shape[2])
            if shape[0] != 128:
                ap = ap[0:shape[0]]
            return ap

        def mm(out, lhsT, rhs, start, stop, r, w, selfsync=False):
            S.op('pe', lambda e: e.matmul(out, lhsT, rhs, start=start, stop=stop), r, w, selfsync=selfsync)

        def tr(out, in_, ident, r, w):
            S.op('pe', lambda e: e.transpose(out, in_, ident), r, w)

        def act(out, in_, func, r, w, **kw):
            S.op('act', lambda e: e.activation(out, in_, func, **kw), r, w)

        def cp(eng, out, in_, r, w):
            if eng == 'act':
                S.op('act', lambda e: e.copy(out, in_), r, w)
            else:
                S.op(eng, lambda e: e.tensor_copy(out, in_), r, w)

        def tt(eng, out, in0, in1, op, r, w):
            S.op(eng, lambda e: e.tensor_tensor(out, in0, in1, op), r, w)

        def ts(eng, out, in0, s1, s2, op0, op1, r, w, **kw):
            if op1 is None:
                S.op(eng, lambda e: e.tensor_scalar(out, in0, s1, None, op0=op0, **kw), r, w)
            else:
                S.op(eng, lambda e: e.tensor_scalar(out, in0, s1, s2, op0=op0, op1=op1, **kw), r, w)

        def stt(eng, out, in0, sc, in1, op0, op1, r, w):
            S.op('dve', lambda e: e.scalar_tensor_tensor(out, in0, sc, in1, op0=op0, op1=op1), r, w)

        def memset(eng, out, val, w):
            S.op(eng, lambda e: e.memset(out, val), (), w)

        def bview(i, n=1024):
            return PB[i].bitcast(BF16)[:, 0:n]

        cst = A([128, NCST])
        S.dma('sp', cst, cst_d[:, :], ['cst_d'], ['cst'])

        def C(n):
            o, w = CST[n]
            return cst[:, o:o + w]
        ident_b = A([128, 128], BF16)
        U_b = A([128, 128], BF16)
        NEGsl4_b = A([128, 512], BF16)
        ones_b = A([128, 128], BF16)
        cp('dve', ident_b, C('ident'), ['cst'], ['ident_b'])
        cp('dve', U_b, C('U'), ['cst'], ['U_b'])
        cp('dve', NEGsl4_b, C('NEGsl4'), ['cst'], ['NEGsl4_b'])
        cp('dve', ones_b, C('ones'), ['cst'], ['ones_b'])
        negn1 = A([128, 1])
        ts('dve', negn1, C('n1col'), -1.0, None, ALU.mult, None, ['cst'], ['negn1'])
        KC = ['cst', 'ident_b', 'U_b', 'NEGsl4_b', 'ones_b', 'negn1']
        P_BASE = st['off']

        def new_phase():
            S.barrier()
            st['off'] = P_BASE

        def rstd_inplace(ap, key, scale):
            ts('dve', ap, ap, scale, EPS, ALU.mult, ALU.add, [key], [key])
            act(ap, ap, AF.Sqrt, [key], [key])
            S.op('dve', lambda e: e.reciprocal(ap, ap), [key], [key])

        def sinred(out, ang, off, n, tmp, key, okey=None):
            okey = okey or (key + 'out')
            r_, k_, f_ = tmp
            ts('dve', r_, ang, 1.0 / TWO_PI, off, ALU.mult, ALU.add, [key + 'ang'], [key + 'r'])
            cp('dve', k_.bitcast(mybir.dt.int32), r_, [key + 'r'], [key + 'k'])
            cp('dve', f_, k_.bitcast(mybir.dt.int32), [key + 'k'], [key + 'f'])
            tt('dve', r_, r_, f_, ALU.subtract, [key + 'r', key + 'f'], [key + 'r'])
            stt('dve', f_, r_, 0.5, r_, ALU.is_gt, ALU.subtract, [key + 'r'], [key + 'f'])
            stt('dve', r_, f_, 0.5, f_, ALU.is_gt, ALU.subtract, [key + 'f'], [key + 'r'])
            act(out, r_, AF.Sin, [key + 'r'], [okey], scale=TWO_PI)

        for l in range(NL):
            xsrc = x_d if l == 0 else xb_d
            xdst = out_d if l == NL - 1 else xb_d
            xin_key = (lambda t: 'xin%d' % t)

            def PPd(n, l=l):
                o, w = PP[n]
                return pp_d[l, :, o:o + w]

            new_phase()
            Win_b = A([128, 8, 1664], BF16)
            Wu5_b = A([128, 8, 256], BF16)
            Wc_b = A([128, 4, 8, 512], BF16)
            ppa = A([128, 8 + 8 + 512 + 128 + 64 + 4 + 4 + 4 + 2])
            NPA = 8 + 8 + 512 + 128 + 64 + 4 + 4 + 4 + 2
            S.dma('sp', ppa, pp_d[l, :, 0:NPA], ['pp_d'], ['ppa'])
            convw = A([128, 4, 512])
            S.dma('sp', convw, PPd('convw').rearrange("p (a b) -> p a b", a=4), ['pp_d'], ['convw'])
            cbrow = A([128, 512])
            S.dma('sp', cbrow, PPd('convb_row'), ['pp_d'], ['cbrow'])
            cbcol = A([128, 2])
            S.dma('sp', cbcol, PPd('convb_col'), ['pp_d'], ['cbcol'])
            cbrow_b = A([128, 384], BF16)
            cp('dve', cbrow_b, cbrow[:, 0:384], ['cbrow'], ['cbrow_b'])

            def PA(n):
                o, w = PP[n]
                return ppa[:, o:o + w]
            aneg = A([128, 4])
            act(aneg, PA('alog'), AF.Exp, ['ppa'], ['aneg'])
            ts('dve', aneg, aneg, -1.0, None, ALU.mult, None, ['aneg'], ['aneg'])
            stg = Rot([A([128, 2380]), A([128, 2380])], 'stg')
            for c in range(8):
                sg_, sk = stg.next()
                S.dma('sp' if c % 2 == 0 else 'pool', sg_, win_d[l, c * 128:(c + 1) * 128, :], ['win_d'], [sk])
                gm = PA('gmix')[:, c:c + 1]
                ts('dve', Win_b[:, c, 0:1356], sg_[:, 0:1356], gm, None, ALU.mult, None, [sk, 'ppa'], ['Win_b'])
                ts('dve', Win_b[:, c, CB_Z:CB_Z + 256], sg_[:, C_Z:C_Z + 256], gm, None, ALU.mult, None, [sk, 'ppa'], ['Win_b'])
                ts('pool', Wu5_b[:, c, :], sg_[:, C_U5:C_U5 + 256], gm, None, ALU.mult, None, [sk, 'ppa'], ['Wu5_b'])
                for tap in range(4):
                    stt('dve' if tap % 2 == 0 else 'pool', Wc_b[:, tap, c, :], sg_[:, C_XBC:C_XBC + 512], gm, convw[:, tap, :],
                        ALU.mult, ALU.mult, [sk, 'ppa', 'convw'], ['Wc_b'])
            xrot = Rot([A([128, D]), A([128, D])], 'xt')
            hrot = Rot([A([128, 8, 128], BF16), A([128, 8, 128], BF16)], 'hT')
            hsh = [A([128, 8, 128], BF16) for _ in range(3)]
            hb = A([128, D], BF16)
            junkA = A([128, D])
            ss = A([128, 1])
            sq = A([128, 512])
            qn = A([128, 512])
            qb = A([128, 512], BF16)
            st8 = A([128, 8])
            rtmp = [A([128, 64]) for _ in range(4)]
            csrot = Rot([A([128, 16]), A([128, 16])], 'cs')
            qTt = Rot([A([128, 4, 128], BF16), A([128, 4, 128], BF16)], 'qTt')
            qiTt = Rot([A([128, 4, 128], BF16), A([128, 4, 128], BF16)], 'qiTt')
            kTt = Rot([A([128, 128], BF16), A([128, 128], BF16)], 'kTt')
            kiTt = Rot([A([64, 128], BF16), A([64, 128], BF16)], 'kiTt')
            vbt = Rot([A([128, 2, 65], BF16), A([128, 2, 65], BF16)], 'vbt')
            for v_, _k in zip(vbt.aps, ('vbt0', 'vbt1')):
                memset('pool', v_, 1.0, [_k])
            sgt = Rot([A([128, 8]), A([128, 8])], 'sgt')
            wabs = A([128, 8])
            wneg = A([128, 8])
            dtt = Rot([A([128, 32]), A([128, 32])], 'dtt')
            for d__, k__ in zip(dtt.aps, ('dtt0', 'dtt1')):
                memset('pool', d__, 0.0, [k__])
            dtmp = [A([128, 4]) for _ in range(3)]
            zst = Rot([A([128, 256]), A([128, 256])], 'zst')
            u5t = Rot([A([128, 2, 128], BF16), A([128, 2, 128], BF16)], 'u5t')
            xsbt = Rot([A([128, 384], BF16), A([128, 384], BF16)], 'xsbt')
            bct = Rot([A([128, 2, 128], BF16), A([128, 2, 128], BF16)], 'bct')

            def rope(src3, dst3, H, cst_, csk, sk, dk, eng='pool'):
                cb = cst_[:, 0:8].unsqueeze(1).to_broadcast([128, H, 8])
                sb_ = cst_[:, 8:16].unsqueeze(1).to_broadcast([128, H, 8])
                x1 = src3[:, :, 0:8]
                x2 = src3[:, :, 8:16]
                t = [r_[:, 0:H * 8].rearrange("p (h j) -> p h j", h=H) for r_ in rtmp]
                tt(eng, t[0], x1, cb, ALU.mult, [sk, csk], ['rt0'])
                tt(eng, t[1], x2, sb_, ALU.mult, [sk, csk], ['rt1'])
                tt(eng, dst3[:, :, 0:8], t[0], t[1], ALU.subtract, ['rt0', 'rt1'], [dk])
                tt(eng, t[2], x2, cb, ALU.mult, [sk, csk], ['rt2'])
                tt(eng, t[3], x1, sb_, ALU.mult, [sk, csk], ['rt3'])
                tt(eng, dst3[:, :, 8:16], t[2], t[3], ALU.add, ['rt2', 'rt3'], [dk])

            def headnorm(ps_ap, psk, H, g_ap):
                n = H * 64
                act(sq[:, 0:n], ps_ap, AF.Square, [psk], ['sq'])
                S.op('dve', lambda e: e.tensor_reduce(st8[:, 0:H], sq[:, 0:n].rearrange("p (h d) -> p h d", h=H), AX.X, ALU.add), ['sq'], ['st8'])
                rstd_inplace(st8[:, 0:H], 'st8', 1.0 / 64)
                tt('dve', qn[:, 0:n].rearrange("p (h d) -> p h d", h=H), ps_ap.rearrange("p (h d) -> p h d", h=H),
                   st8[:, 0:H].unsqueeze(2).to_broadcast([128, H, 64]), ALU.mult, [psk, 'st8'], ['qn'])
                tt('pool', qn[:, 0:n], qn[:, 0:n], g_ap, ALU.mult, ['qn', 'ppa'], ['qn'])

            for t in range(NT if stop != 'A0' else 0):
                tok = slice(t * 128, (t + 1) * 128)
                xt, xk = xrot.next()
                S.dma('sp', xt, xsrc[tok, :], [xin_key(t)], [xk])
                cst_, csk = csrot.next()
                S.dma('pool', cst_, cs_d[tok, :], ['cs_d'], [csk])
                memset('pool', ss, 0.0, ['ss'])
                act(junkA, xt, AF.Square, [xk], ['junkA', 'ss'], accum_out=ss)
                rstd_inplace(ss, 'ss', 1.0 / D)
                ts('dve', hb, xt, ss, None, ALU.mult, None, [xk, 'ss'], ['hb'])
                hT, hk = hrot.next()
                pT = bview(7)
                for c in range(8):
                    tr(pT[:, c * 128:(c + 1) * 128], hb[:, c * 128:(c + 1) * 128], ident_b, ['hb', 'ident_b'], [PBk[7]])
                cp('act', hT, pT.rearrange("p (c t) -> p c t", c=8), [PBk[7]], [hk])
                hp, hpk = hrot.prev()
                for tap in range(3):
                    nh = 3 - tap
                    e_ = 'pool' if tap % 2 == 0 else 'act'
                    if t == 0:
                        memset('pool', hsh[tap][:, :, 0:nh], 0.0, ['hsh%d' % tap])
                    else:
                        cp('pool', hsh[tap][:, :, 0:nh], hp[:, :, 128 - nh:128], [hpk], ['hsh%d' % tap])
                    cp(e_, hsh[tap][:, :, nh:128], hT[:, :, 0:128 - nh], [hk], ['hsh%d' % tap])
                H = hsh + [hT]
                Hk = ['hsh0', 'hsh1', 'hsh2', hk]
                hc = [hT[:, c, :] for c in range(8)]
                if stop == 'A1':
                    continue
                for c in range(8):
                    mm(PB[2][:, 0:332], hc[c], Win_b[:, c, C_KV:C_KV + 332], c == 0, c == 7, [hk, 'Win_b'], [PBk[2]])
                ts('dve', wabs, PB[2][:, 320:328], IDX_W_SCALE, None, ALU.mult, None, [PBk[2]], ['wabs'])
                ts('dve', wneg, PB[2][:, 320:328], -IDX_W_SCALE, None, ALU.mult, None, [PBk[2]], ['wneg'])
                tt('dve', wabs, wabs, wneg, ALU.max, ['wabs', 'wneg'], ['wabs'])
                sgx, sgk = sgt.next()
                ts('dve', sgx, PB[2][:, 320:328], 0.0, 2.0, ALU.is_ge, ALU.mult, [PBk[2]], [sgk])
                ts('dve', sgx, sgx, -1.0, None, ALU.add, None, [sgk], [sgk])
                S.dma('sp', sg_s[tok, :], sgx, [sgk], ['sg_s%d' % t])
                dtx, dtk = dtt.next()
                tt('dve', dtmp[0], PB[2][:, 328:332], PA('dtb'), ALU.add, [PBk[2], 'ppa'], ['dtmp0'])
                ts('dve', dtmp[2], dtmp[0], -1.0, None, ALU.mult, None, ['dtmp0'], ['dtmp2'])
                tt('dve', dtmp[1], dtmp[0], dtmp[2], ALU.max, ['dtmp0', 'dtmp2'], ['dtmp1'])
                act(dtmp[1], dtmp[1], AF.Exp, ['dtmp1'], ['dtmp1'], scale=-1.0)
                act(dtmp[1], dtmp[1], AF.Ln, ['dtmp1'], ['dtmp1'], bias=C('ones')[:, 0:1])
                stt('dve', dtx[:, 0:4], dtmp[0], 0.0, dtmp[1], ALU.max, ALU.add, ['dtmp0', 'dtmp1'], [dtk])
                tt('dve', dtx[:, 16:20], dtx[:, 0:4], aneg, ALU.mult, [dtk, 'aneg'], [dtk])
                S.dma('sp', dt_s[tok, :], dtx, [dtk], ['dt_s%d' % t])
                vb, vk = vbt.next()
                cp('act', vb[:, :, 0:64], PB[2][:, 128:256].rearrange("p (g d) -> p g d", g=2), [PBk[2]], [vk])
                S.dma('sp', v_s[tok, :], vb.rearrange("p g d -> p (g d)"), [vk], ['v_s%d' % t])
                headnorm(PB[2][:, 0:128], PBk[2], 2, PA('gk'))
                cp('act', qb[:, 0:128], qn[:, 0:128], ['qn'], ['qb'])
                rope(qn[:, 0:128].rearrange("p (h d) -> p h d", h=2), qb[:, 0:128].rearrange("p (h d) -> p h d", h=2), 2, cst_, csk, 'qn', 'qb')
                pk = PB[5].bitcast(BF16)[:, 512:640]
                tr(pk, qb[:, 0:128], ident_b, ['qb', 'ident_b'], [PBk[5]])
                kx, kk = kTt.next()
                cp('act', kx, pk, [PBk[5]], [kk])
                S.dma('sp', kT_s[:, tok], kx, [kk], ['kT_s%d' % t])
                headnorm(PB[2][:, 256:320], PBk[2], 1, PA('gki'))
                cp('act', qb[:, 0:64], qn[:, 0:64], ['qn'], ['qb'])
                rope(qn[:, 0:64].rearrange("p (h d) -> p h d", h=1), qb[:, 0:64].rearrange("p (h d) -> p h d", h=1), 1, cst_, csk, 'qn', 'qb')
                pki = PB[5].bitcast(BF16)[0:64, 640:768]
                tr(pki, qb[:, 0:64], ident_b, ['qb', 'ident_b'], [PBk[5]])
                kix, kik = kiTt.next()
                cp('act', kix, pki, [PBk[5]], [kik])
                S.dma('sp', kiT_s[:, tok], kix, [kik], ['kiT_s%d' % t])
                if stop == 'A2':
                    continue
                for c in range(8):
                    mm(PB[0][:, 0:512], hc[c], Win_b[:, c, C_Q:C_Q + 512], c == 0, c == 7, [hk, 'Win_b'], [PBk[0]])
                headnorm(PB[0][:, 0:512], PBk[0], 8, PA('gq'))
                cp('act', qb, qn, ['qn'], ['qb'])
                rope(qn.rearrange("p (h d) -> p h d", h=8), qb.rearrange("p (h d) -> p h d", h=8), 8, cst_, csk, 'qn', 'qb')
                pq = bview(6)
                for j in range(4):
                    tr(pq[:, j * 128:(j + 1) * 128], qb[:, j * 128:(j + 1) * 128], ident_b, ['qb', 'ident_b'], [PBk[6]])
                qx, qk_ = qTt.next()
                cp('act', qx, pq[:, 0:512].rearrange("p (j t) -> p j t", j=4), [PBk[6]], [qk_])
                S.dma('pool', qT_s[:, :, tok].rearrange("j p t -> p j t"), qx, [qk_], ['qT_s%d' % t])
                for c in range(8):
                    mm(PB[1][:, 0:512], hc[c], Win_b[:, c, C_QI:C_QI + 512], c == 0, c == 7, [hk, 'Win_b'], [PBk[1]])
                tt('dve', qn.rearrange("p (h d) -> p h d", h=8), PB[1][:, 0:512].rearrange("p (h d) -> p h d", h=8),
                   wabs.unsqueeze(2).to_broadcast([128, 8, 64]), ALU.mult, [PBk[1], 'wabs'], ['qn'])
                cp('act', qb, qn, ['qn'], ['qb'])
                rope(qn.rearrange("p (h d) -> p h d", h=8), qb.rearrange("p (h d) -> p h d", h=8), 8, cst_, csk, 'qn', 'qb')
                for j in range(4):
                    tr(pq[:, 512 + j * 128:512 + (j + 1) * 128], qb[:, j * 128:(j + 1) * 128], ident_b, ['qb', 'ident_b'], [PBk[6]])
                qix, qik = qiTt.next()
                cp('act', qix, pq[:, 512:1024].rearrange("p (j t) -> p j t", j=4), [PBk[6]], [qik])
                S.dma('pool', qiT_s[:, :, tok].rearrange("j p t -> p j t"), qix, [qik], ['qiT_s%d' % t])
                if stop == 'A3':
                    continue
                if stop != 'A4b':
                    for c in range(8):
                        mm(PB[3][:, 0:256], hc[c], Win_b[:, c, CB_Z:CB_Z + 256], c == 0, c == 7, [hk, 'Win_b'], [PBk[3]])
                    zx, zk = zst.next()
                    if stop != 'A4a1':
                        cp('act', zx, PB[3][:, 0:256], [PBk[3]], [zk])
                        if stop != 'A4a2':
                            S.dma('pool' if stop == 'A4a3' else 'sp', zs_s[tok, :], zx, [zk], ['zs_s%d' % t])
                if stop in ('A4a', 'A4a1', 'A4a2', 'A4a3'):
                    continue
                for cc in range(2):
                    for c in range(8):
                        mm(PB[3][:, 256 + cc * 128:256 + (cc + 1) * 128], Wu5_b[:, c, cc * 128:(cc + 1) * 128], hc[c], c == 0, c == 7,
                           [hk, 'Wu5_b'], [PBk[3]])
                ux, uk = u5t.next()
                cp('act', ux, PB[3][:, 256:512].rearrange("p (c t) -> p c t", c=2), [PBk[3]], [uk])
                S.dma('pool', u5T_s[:, :, tok].rearrange("c p t -> p c t"), ux, [uk], ['u5T_s%d' % t])
                if stop in ('A4', 'A4b'):
                    continue
                n_ = 0
                for tap in range(4):
                    for c in range(8):
                        mm(PB[4][:, 0:384], H[tap][:, c, :], Wc_b[:, tap, c, 0:384], n_ == 0, False, [Hk[tap], 'Wc_b'], [PBk[4]])
                        n_ += 1
                mm(PB[4][:, 0:384], ones_b[0:1, :], cbrow_b[0:1, :], False, True, ['ones_b', 'cbrow_b'], [PBk[4]])
                xx, xsk = xsbt.next()
                act(xx, PB[4][:, 0:384], AF.Silu, [PBk[4]], [xsk])
                S.dma('sp', xsb_s[tok, :], xx, [xsk], ['xsb_s%d' % t])
                if stop == 'A5':
                    continue
                for cc in range(2):
                    n_ = 0
                    for tap in range(4):
                        for c in range(8):
                            mm(PB[5][:, cc * 128:(cc + 1) * 128], Wc_b[:, tap, c, 256 + cc * 128:256 + (cc + 1) * 128], H[tap][:, c, :],
                               n_ == 0, n_ == 31, [Hk[tap], 'Wc_b'], [PBk[5]])
                            n_ += 1
                bx, bk = bct.next()
                for cc in range(2):
                    act(bx[:, cc, :], PB[5][:, cc * 128:(cc + 1) * 128], AF.Silu, [PBk[5], 'cbcol'], [bk], bias=cbcol[:, cc:cc + 1])
                S.dma('pool', bcT_s[:, :, tok].rearrange("c p t -> p c t"), bx, [bk], ['bcT_s%d' % t])

            if stop is not None and stop.startswith('A'):
                break
            phase_s5(S, locals())
            if stop in ('s5', 's5a', 's5b'):
                break
            phase_ssd(S, locals())
            if stop is not None and stop.startswith('ssd'):
                break
            phase_attn(S, locals())
            if stop == 'attn':
                break
            phase_ffn(S, locals())
        S.finish()
    return nc


class _NS:
    def __init__(self, d):
        self.__dict__.update(d)


def phase_s5(S, Ld):
    L = _NS(Ld)
    A, mm, tr, act, cp, tt, ts, stt, memset, bview = L.A, L.mm, L.tr, L.act, L.cp, L.tt, L.ts, L.stt, L.memset, L.bview
    PB, PBk, C, PPd, l, NT = L.PB, L.PBk, L.C, L.PPd, L.l, L.NT
    ident_b, U_b = L.ident_b, L.U_b
    L.new_phase()
    lr = A([128, 1024]); li = A([128, 1024]); ls = A([128, 1024])
    S.dma('sp', lr, PPd('lr_row'), ['pp_d'], ['lr'])
    S.dma('pool', li, PPd('li_row'), ['pp_d'], ['li'])
    S.dma('sp', ls, PPd('ls_row'), ['pp_d'], ['ls'])
    act(ls, ls, AF.Exp, ['ls'], ['ls'])
    tt('dve', lr, lr, ls, ALU.mult, ['lr', 'ls'], ['lr'])
    tt('dve', li, li, ls, ALU.mult, ['li', 'ls'], ['li'])
    mag1 = A([128, 1024]); mag2 = A([128, 1024])
    act(mag2, lr, AF.Exp, ['lr', 'cst'], ['mag2'], scale=C('n1col'))
    act(mag1, lr, AF.Exp, ['lr', 'negn1'], ['mag1'], scale=L.negn1)
    ang = A([128, 1024])
    ts('dve', ang, li, C('n1col'), None, ALU.mult, None, ['li', 'cst'], ['Tang'])
    tmp = [A([128, 1024]) for _ in range(3)]
    sinT = A([128, 1024]); cosT = A([128, 1024])
    L.sinred(sinT, ang, 16.0, 1024, tmp, 'T', 'Tsin')
    L.sinred(cosT, ang, 16.25, 1024, tmp, 'T', 'Tout')
    T1re = A([128, 1024]); T1im = A([128, 1024]); T2re = A([128, 1024]); T2im = A([128, 1024])
    tt('dve', T2re, mag2, cosT, ALU.mult, ['mag2', 'Tout'], ['T2'])
    tt('dve', T2im, mag2, sinT, ALU.mult, ['mag2', 'Tsin'], ['T2'])
    tt('dve', T1re, mag1, cosT, ALU.mult, ['mag1', 'Tout'], ['T1'])
    stt('dve', T1im, mag1, -1.0, sinT, ALU.mult, ALU.mult, ['mag1', 'Tsin'], ['T1'])
    pb_ = A([128, 7 * 128])
    S.dma('sp', pb_, L.pp_d[l, :, PP['lrB'][0]:PP['lrB'][0] + 7 * 128], ['pp_d'], ['pb_'])
    lrB, liB, lsB, bre, bim, cre, cim = [pb_[:, i * 128:(i + 1) * 128] for i in range(7)]
    w = [A([128, 128]) for _ in range(12)]
    act(w[0], lsB, AF.Exp, ['pb_'], ['w0'])
    tt('dve', w[1], lrB, w[0], ALU.mult, ['pb_', 'w0'], ['w1'])
    tt('dve', w[2], liB, w[0], ALU.mult, ['pb_', 'w0'], ['Bang'])
    act(w[1], w[1], AF.Exp, ['w1'], ['w1'])
    L.sinred(w[3], w[2], 16.0, 128, [w[4], w[5], w[6]], 'B')
    S.op('dve', lambda e: e.tensor_copy(w[7], w[3]), ['Bout'], ['Bsin'])
    L.sinred(w[3], w[2], 16.25, 128, [w[4], w[5], w[6]], 'B')
    tt('dve', w[4], w[1], w[3], ALU.mult, ['w1', 'Bout'], ['abre'])
    tt('dve', w[5], w[1], w[7], ALU.mult, ['w1', 'Bsin'], ['abim'])
    ts('dve', w[4], w[4], -1.0, None, ALU.add, None, ['abre'], ['abre'])
    tt('dve', w[6], lrB, lrB, ALU.mult, ['pb_'], ['w6'])
    tt('dve', w[8], liB, liB, ALU.mult, ['pb_'], ['w8'])
    tt('dve', w[6], w[6], w[8], ALU.add, ['w6', 'w8'], ['w6'])
    S.op('dve', lambda e: e.reciprocal(w[6], w[6]), ['w6'], ['w6'])
    tt('dve', w[8], w[4], lrB, ALU.mult, ['abre', 'pb_'], ['w8'])
    tt('dve', w[9], w[5], liB, ALU.mult, ['abim', 'pb_'], ['w9'])
    tt('dve', w[8], w[8], w[9], ALU.add, ['w8', 'w9'], ['w8'])
    tt('dve', w[8], w[8], w[6], ALU.mult, ['w8', 'w6'], ['cr'])
    tt('dve', w[9], w[5], lrB, ALU.mult, ['abim', 'pb_'], ['w9'])
    tt('dve', w[10], w[4], liB, ALU.mult, ['abre', 'pb_'], ['w10'])
    tt('dve', w[9], w[9], w[10], ALU.subtract, ['w9', 'w10'], ['w9'])
    tt('dve', w[9], w[9], w[6], ALU.mult, ['w9', 'w6'], ['ci'])
    tt('dve', w[0], w[8], bre, ALU.mult, ['cr', 'pb_'], ['w0'])
    tt('dve', w[1], w[9], bim, ALU.mult, ['ci', 'pb_'], ['w1'])
    tt('dve', w[0], w[0], w[1], ALU.subtract, ['w0', 'w1'], ['bbre'])
    tt('dve', w[2], w[8], bim, ALU.mult, ['cr', 'pb_'], ['w2'])
    tt('dve', w[3], w[9], bre, ALU.mult, ['ci', 'pb_'], ['w3'])
    tt('dve', w[2], w[2], w[3], ALU.add, ['w2', 'w3'], ['bbim'])
    Bblk = A([128, 4, 512], BF16)
    bmask = C('bmask').rearrange("p (g q) -> p g q", g=8)
    for part, (bb, bbk) in enumerate(((w[0], 'bbre'), (w[2], 'bbim'))):
        for c in range(2):
            tt('dve', Bblk[:, part * 2 + c, :].rearrange("p (g q) -> p g q", g=8), bmask,
               bb[:, c * 64:(c + 1) * 64].unsqueeze(1).to_broadcast([128, 8, 64]), ALU.mult, [bbk, 'cst'], ['Bblk'])
    Cblk = A([128, 16, 32], BF16)
    cmask = C('cmask').rearrange("p (g h) -> p g h", g=2)
    cre3 = cre.rearrange("p (j h) -> p j h", j=8)
    cim3 = cim.rearrange("p (j h) -> p j h", j=8)
    for g2 in range(2):
        cmb = cmask[:, g2, :].unsqueeze(1).to_broadcast([128, 8, 16])
        tt('dve', Cblk[:, 0:8, 16 * g2:16 * (g2 + 1)], cre3, cmb, ALU.mult, ['pb_', 'cst'], ['Cblk'])
        stt('dve', Cblk[:, 8:16, 16 * g2:16 * (g2 + 1)], cim3, -1.0, cmb, ALU.mult, ALU.mult, ['pb_', 'cst'], ['Cblk'])
    dg2 = A([128, 4]); S.dma('sp', dg2, L.pp_d[l, :, PP['dcol'][0]:PP['dcol'][0] + 4], ['pp_d'], ['dg2'])
    dgd = A([128, 2, 128], BF16)
    for c in range(2):
        ts('dve', dgd[:, c, :], C('ident'), dg2[:, c:c + 1], None, ALU.mult, None, ['cst', 'dg2'], ['dgd'])
    glf = A([128, 2, 256])
    S.dma('sp', glf, L.glu_d[l].rearrange("(c p) n -> p c n", p=128), ['glu_d'], ['glf'])
    glb = A([128, 2, 256], BF16)
    cp('dve', glb, glf, ['glf'], ['glb'])
    uT = Rot([A([128, 2, 128], BF16), A([128, 2, 128], BF16)], 'uT')
    V = A([128, 2048], BF16)
    ta = [A([128, 512]) for _ in range(4)]
    X = Rot([A([128, 2048]), A([128, 2048])], 'X')
    Xb = A([128, 2048], BF16)
    XT = A([128, 16, 128], BF16)
    y2b = A([128, 256], BF16)
    y2T = A([128, 2, 128], BF16)
    sgm = A([128, 2, 128])
    s5o = Rot([A([128, 2, 128], BF16), A([128, 2, 128], BF16)], 's5o')
    Ts = {'T1re': T1re, 'T1im': T1im, 'T2re': T2re, 'T2im': T2im}

    def cmul(re_ps, im_ps, rk, ik, Tre, Tim, Tk, j, out_re, out_im, ok):
        cs_ = slice(512 * j, 512 * (j + 1))
        tt('dve', ta[0], re_ps, Tre[:, cs_], ALU.mult, [rk, Tk], ['ta0'])
        tt('dve', ta[1], im_ps, Tim[:, cs_], ALU.mult, [ik, Tk], ['ta1'])
        tt('pool', out_re, ta[0], ta[1], ALU.subtract, ['ta0', 'ta1'], [ok])
        tt('dve', ta[2], im_ps, Tre[:, cs_], ALU.mult, [ik, Tk], ['ta2'])
        tt('dve', ta[3], re_ps, Tim[:, cs_], ALU.mult, [rk, Tk], ['ta3'])
        tt('pool', out_im, ta[2], ta[3], ALU.add, ['ta2', 'ta3'], [ok])

    for t in range(NT if L.stop != 's5a' else 0):
        tok = slice(t * 128, (t + 1) * 128)
        u, uk = uT.next()
        S.dma('sp', u, L.u5T_s[:, :, tok].rearrange("c p t -> p c t"), ['u5T_s%d' % t], [uk])
        for part in range(2):
            for c in range(2):
                b = part * 2 + c
                mm(PB[b][:, 0:512], u[:, c, :], Bblk[:, b, :], True, True, [uk, 'Bblk'], [PBk[b]])
        for j in range(2):
            cmul(PB[j][:, 0:512], PB[2 + j][:, 0:512], PBk[j], PBk[2 + j], T1re, T1im, 'T1', j,
                 V[:, 512 * j:512 * (j + 1)], V[:, 1024 + 512 * j:1024 + 512 * (j + 1)], 'V')
        x, xk = X.next()
        xp, xpk = X.prev()
        for b in range(4):
            mm(PB[4 + b][:, 0:512], U_b, V[:, 512 * b:512 * (b + 1)], True, t == 0, ['V', 'U_b'], [PBk[4 + b]])
            if t > 0:
                mm(PB[4 + b][:, 0:512], C('Sel'), xp[:, 512 * b:512 * (b + 1)], False, True, [xpk, 'cst'], [PBk[4 + b]])
        for j in range(2):
            cmul(PB[4 + j][:, 0:512], PB[6 + j][:, 0:512], PBk[4 + j], PBk[6 + j], T2re, T2im, 'T2', j,
                 x[:, 512 * j:512 * (j + 1)], x[:, 1024 + 512 * j:1024 + 512 * (j + 1)], xk)
        cp('act', Xb, x, [xk], ['Xb'])
        for blk in range(16):
            pv = bview(blk // 8)
            tr(pv[:, (blk % 8) * 128:(blk % 8 + 1) * 128], Xb[:, blk * 128:(blk + 1) * 128], ident_b, ['Xb', 'ident_b'], [PBk[blk // 8]])
        for hf in range(2):
            cp('act', XT[:, hf * 8:(hf + 1) * 8, :], bview(hf).rearrange("p (j t) -> p j t", j=8), [PBk[hf]], ['XT'])
        for c in range(2):
            mm(PB[2][:, c * 128:(c + 1) * 128], u[:, c, :], dgd[:, c, :], True, False, [uk, 'dgd'], [PBk[2]])
            for m in range(4 * c, 4 * c + 4):
                for part in range(2):
                    j = m + 8 * part
                    mm(PB[2][:, 32 * m:32 * m + 32], XT[:, j, :], Cblk[:, j, :], False, (m == 4 * c + 3 and part == 1), ['XT', 'Cblk'], [PBk[2]])
        act(y2b, PB[2][:, 0:256], AF.Gelu_apprx_tanh, [PBk[2]], ['y2b'])
        pv = PB[3].bitcast(BF16)[:, 0:256]
        for c in range(2):
            tr(pv[:, c * 128:(c + 1) * 128], y2b[:, c * 128:(c + 1) * 128], ident_b, ['y2b', 'ident_b'], [PBk[3]])
        cp('act', y2T, pv.rearrange("p (c t) -> p c t", c=2), [PBk[3]], ['y2T'])
        for co in range(2):
            for c in range(2):
                mm(PB[3][:, 128 + 128 * co:256 + 128 * co], glb[:, c, co * 128:(co + 1) * 128], y2T[:, c, :], c == 0, c == 1,
                   ['glb', 'y2T'], [PBk[3]])
        for co in range(2):
            act(sgm[:, co, :], PB[3][:, 128 + 128 * co:256 + 128 * co], AF.Sigmoid, [PBk[3], 'dg2'], ['sgm'], bias=dg2[:, 2 + co:3 + co])
        so, sok = s5o.next()
        tt('dve', so, y2T, sgm, ALU.mult, ['y2T', 'sgm'], [sok])
        S.dma('pool', L.catT_s[4:6, :, tok].rearrange("c p t -> p c t"), so, [sok], ['catT_s5_%d' % t])


def phase_ssd(S, Ld):
    L = _NS(Ld)
    A, mm, tr, act, cp, tt, ts, stt, memset, bview = L.A, L.mm, L.tr, L.act, L.cp, L.tt, L.ts, L.stt, L.memset, L.bview
    PB, PBk, C, l, NT = L.PB, L.PBk, L.C, L.l, L.NT
    ident_b = L.ident_b
    L.new_phase()
    pps = A([128, 16])
    o0 = PP['dtb'][0]
    S.dma('sp', pps[:, 0:14], L.pp_d[l, :, o0:o0 + 14], ['pp_d'], ['pps'])
    dsk = pps[:, 8:12]
    xsb = Rot([A([128, 384], BF16), A([128, 384], BF16)], 'xsb')
    bcT = Rot([A([128, 2, 128], BF16), A([128, 2, 128], BF16)], 'bcT')
    zs = Rot([A([128, 256]), A([128, 256])], 'zs')
    dts = Rot([A([128, 32]), A([128, 32])], 'dts')
    rhs4 = A([128, 4, 128])
    E4 = A([128, 4, 128])
    acs8 = A([128, 8])
    ea = A([128, 4]); es = A([128, 4]); et = A([128, 4]); wdt = A([128, 4])
    mtmp = A([128, 128])
    GTs = A([128, 256])
    stS = A([128, 128])
    MTb = A([128, 4, 128], BF16)
    Bw2 = A([128, 4, 128], BF16)
    memset('pool', Bw2, 0.0, ['Bw2'])
    prevT = A([128, 2, 64])
    prevTb = A([128, 2, 64], BF16)
    memset('pool', prevT, 0.0, ['prevT'])
    memset('pool', prevTb, 0.0, ['prevTb'])
    yt = A([128, 256]); y = A([128, 256]); sz = A([128, 256]); junk = A([128, 128])
    ssg = A([128, 2])
    yb = A([128, 256], BF16)
    so = Rot([A([128, 2, 128], BF16), A([128, 2, 128], BF16)], 'sso')
    for t in range(NT):
        tok = slice(t * 128, (t + 1) * 128)
        xs_, xsk = xsb.next()
        S.dma('sp', xs_, L.xsb_s[tok, :], ['xsb_s%d' % t], [xsk])
        bc, bck = bcT.next()
        S.dma('pool', bc, L.bcT_s[:, :, tok].rearrange("c p t -> p c t"), ['bcT_s%d' % t], [bck])
        z_, zk = zs.next()
        S.dma('sp', z_, L.zs_s[tok, :], ['zs_s%d' % t], [zk])
        d_, dk = dts.next()
        S.dma('pool', d_, L.dt_s[tok, :], ['dt_s%d' % t], [dk])
        dt_ = d_[:, 0:4]
        adt = d_[:, 16:20]
        for h in range(4):
            ts('dve', rhs4[:, h, :], C('U'), adt[:, h:h + 1], None, ALU.mult, None, ['cst', dk], ['rhs4'])
        mm(PB[0][:, 0:512], ident_b, L.NEGsl4_b, True, False, ['ident_b', 'NEGsl4_b'], [PBk[0]])
        for h in range(4):
            mm(PB[0][:, 128 * h:128 * (h + 1)], C('M1'), rhs4[:, h, :], False, h == 3, ['cst', 'rhs4'], [PBk[0]])
        act(E4.rearrange("p h l -> p (h l)"), PB[0][:, 0:512], AF.Exp, [PBk[0]], ['E4'])
        if L.stop == 'ssd1':
            continue
        mm(PB[1][:, 0:4], C('U'), adt, True, True, ['cst', dk], [PBk[1]])
        mm(PB[1][:, 4:8], C('ones'), adt, True, True, ['cst', dk], [PBk[1]])
        cp('act', acs8, PB[1][:, 0:8], [PBk[1]], ['acs8'])
        act(ea, acs8[:, 0:4], AF.Exp, ['acs8'], ['ea'])
        act(et, acs8[:, 4:8], AF.Exp, ['acs8'], ['et'])
        tt('dve', es, acs8[:, 4:8], acs8[:, 0:4], ALU.subtract, ['acs8'], ['es'])
        act(es, es, AF.Exp, ['es'], ['es'])
        tt('dve', wdt, es, dt_, ALU.mult, ['es', dk], ['wdt'])
        if L.stop == 'ssd2':
            continue
        if L.stop != 'ssd2c':
            for g in range(2):
                mm(PB[2][:, 128 * g:128 * (g + 1)], bc[64 * g:64 * g + 64, 0, :], bc[64 * g:64 * g + 64, 1, :], True, True, [bck], [PBk[2]], selfsync=True)
            cp('act', GTs, PB[2][:, 0:256], [PBk[2]], ['GTs'])
        else:
            memset('pool', GTs, 1.0, ['GTs'])
        if L.stop == 'ssd2b':
            continue
        for h in range(4):
            g = h // 2
            stt('dve', mtmp, GTs[:, 128 * g:128 * (g + 1)], dt_[:, h:h + 1], E4[:, h, :], ALU.mult, ALU.mult, ['GTs', dk, 'E4'], ['mtmp'])
            stt('dve', MTb[:, h, :], C('ident'), dsk[:, h:h + 1], mtmp, ALU.mult, ALU.add, ['cst', 'pps', 'mtmp'], ['MTb'])
        if L.stop == 'ssd3':
            continue
        for h in range(4):
            mm(PB[3][:, 64 * h:64 * (h + 1)], MTb[:, h, :], xs_[:, 64 * h:64 * (h + 1)], True, True, ['MTb', xsk], [PBk[3]])
        for h in range(4):
            g, r = h // 2, h % 2
            mm(PB[4][:, 64 * h:64 * (h + 1)], bc[64 * g:64 * g + 64, 1, :], prevTb[64 * g:64 * g + 64, r, :], True, True, [bck, 'prevTb'], [PBk[4]], selfsync=True)
        tt('dve', yt.rearrange("p (h q) -> p h q", h=4), PB[4][:, 0:256].rearrange("p (h q) -> p h q", h=4),
           ea.unsqueeze(2).to_broadcast([128, 4, 64]), ALU.mult, [PBk[4], 'ea'], ['yt'])
        tt('dve', y, yt, PB[3][:, 0:256], ALU.add, ['yt', PBk[3]], ['y'])
        if L.stop == 'ssd4':
            continue
        for h in range(4):
            g = h // 2
            ts('pool', Bw2[:, h, 64 * g:64 * g + 64], xs_[:, 256 + 64 * g:256 + 64 * g + 64], wdt[:, h:h + 1], None, ALU.mult, None,
               [xsk, 'wdt'], ['Bw2'])
        for r in range(2):
            for g in range(2):
                h = 2 * g + r
                mm(PB[5][:, 64 * r:64 * (r + 1)], Bw2[:, h, :], xs_[:, 64 * h:64 * (h + 1)], g == 0, g == 1, ['Bw2', xsk], [PBk[5]])
        cp('act', stS, PB[5][:, 0:128], [PBk[5]], ['stS'])
        for r in range(2):
            for g in range(2):
                h = 2 * g + r
                ps_ = slice(64 * g, 64 * g + 64)
                stt('dve', prevT[ps_, r, :], prevT[ps_, r, :], et[ps_, h:h + 1], stS[ps_, 64 * r:64 * (r + 1)], ALU.mult, ALU.add,
                    ['prevT', 'et', 'stS'], ['prevT'])
        cp('pool', prevTb, prevT, ['prevT'], ['prevTb'])
        if L.stop == 'ssd5':
            continue
        act(sz, z_, AF.Silu, [zk], ['sz'])
        tt('dve', y, y, sz, ALU.mult, ['y', 'sz'], ['y'])
        memset('pool', ssg, 0.0, ['ssg'])
        for g in range(2):
            act(junk, y[:, 128 * g:128 * (g + 1)], AF.Square, ['y'], ['junkS', 'ssg'], accum_out=ssg[:, g:g + 1])
        L.rstd_inplace(ssg, 'ssg', 1.0 / 128)
        tt('dve', yb.rearrange("p (g q) -> p g q", g=2), y.rearrange("p (g q) -> p g q", g=2),
           ssg.unsqueeze(2).to_broadcast([128, 2, 128]), ALU.mult, ['y', 'ssg'], ['yb'])
        pv = PB[6].bitcast(BF16)[:, 0:256]
        for c in range(2):
            tr(pv[:, c * 128:(c + 1) * 128], yb[:, c * 128:(c + 1) * 128], ident_b, ['yb', 'ident_b'], [PBk[6]])
        o_, ok_ = so.next()
        cp('act', o_, pv.rearrange("p (c t) -> p c t", c=2), [PBk[6]], [ok_])
        S.dma('sp', L.catT_s[6:8, :, tok].rearrange("c p t -> p c t"), o_, [ok_], ['catT_ssd_%d' % t])


def phase_attn(S, Ld):
    L = _NS(Ld)
    A, mm, tr, act, cp, tt, ts, stt, memset, bview = L.A, L.mm, L.tr, L.act, L.cp, L.tt, L.ts, L.stt, L.memset, L.bview
    PB, PBk, C, l, NT, SQ, NQB, TOPK = L.PB, L.PBk, L.C, L.l, L.NT, L.SQ, L.NQB, L.TOPK
    ident_b = L.ident_b
    L.new_phase()
    kT2 = A([128, SQ], BF16)
    kiT2 = A([128, SQ], BF16)
    Vt = A([128, NT, 130], BF16)
    S.dma('sp', kT2, L.kT_s[:, :], ['kT_s%d' % t for t in range(NT)], ['kT2'])
    for hf in range(2):
        S.dma('sp' if hf == 0 else 'pool', kiT2[64 * hf:64 * hf + 64, :], L.kiT_s[:, :], ['kiT_s%d' % t for t in range(NT)], ['kiT2'])
    v3 = L.v_s.rearrange("(t p) c -> p t c", p=128)
    for t0 in range(0, NT, 8):
        t1 = min(NT, t0 + 8)
        S.dma('sp' if (t0 // 8) % 2 == 0 else 'pool', Vt[:, t0:t1, :], v3[:, t0:t1, :], ['v_s%d' % t for t in range(t0, t1)], ['Vt'])
    qT = A([128, 4, 512], BF16)
    qiT = A([128, 4, 512], BF16)
    sgn = A([128, 4, 8])
    dg = Rot([A([128, 8, 128], BF16), A([128, 8, 128], BF16)], 'dg')
    Rr = Rot([A([128, 512], BF16) for _ in range(3)], 'R')
    sc = A([128, SQ])
    NKB = NT
    maskT = A([128, NKB, 512], BF16)
    mrow = A([128, SQ], BF16)
    lo = A([128, 1]); w0 = A([128, 1]); mid = A([128, 1]); cnt = A([128, 1]); inc = A([128, 1]); hi = A([128, 1])
    wtab = A([128, NIT])
    Er = Rot([A([128, 512], BF16) for _ in range(3)], 'E')
    rden = A([128, 512])
    bcs = A([64, 512])
    Ob = Rot([A([64, 512], BF16), A([64, 512], BF16)], 'Ob')
    psr = Rot([PB[0], PB[1], PB[2], PB[3]], PBk[0:4])
    pacc = Rot([PB[4], PB[5]], PBk[4:6])
    for Q in range(NQB):
        qs = slice(Q * 512, (Q + 1) * 512)
        S.dma('sp', qT, L.qT_s[:, :, qs].rearrange("j p t -> p j t"), ['qT_s%d' % t for t in range(4 * Q, 4 * Q + 4)], ['qT'])
        S.dma('pool', qiT, L.qiT_s[:, :, qs].rearrange("j p t -> p j t"), ['qiT_s%d' % t for t in range(4 * Q, 4 * Q + 4)], ['qiT'])
        S.dma('sp', sgn, L.sg_s[qs, :].rearrange("(i p) h -> p i h", p=128), ['sg_s%d' % t for t in range(4 * Q, 4 * Q + 4)], ['sgn'])
        memset('pool', maskT[:, 4 * Q:4 * Q + 4, :], -30000.0, ['maskT'])
        for i in range(4):
            qt = 4 * Q + i
            Lk = 128 * (qt + 1)
            d_, dgk = dg.next()
            for h in range(8):
                ts('pool', d_[:, h, :], ident_b, sgn[:, i, h:h + 1], None, ALU.mult, None, ['ident_b', 'sgn'], [dgk])
            nkc = (Lk + 511) // 512
            for kc in range(nkc):
                wd = min(512, Lk - 512 * kc)
                ks = slice(512 * kc, 512 * kc + wd)
                pa, pak = pacc.next()
                for h in range(8):
                    pr, prk = psr.next()
                    hp = slice(64 * (h % 2), 64 * (h % 2) + 64)
                    mm(pr[:, 0:wd], qiT[hp, h // 2, 128 * i:128 * (i + 1)], kiT2[hp, ks], True, True, ['qiT', 'kiT2'], [prk])
                    R, Rk = Rr.next()
                    act(R[:, 0:wd], pr[:, 0:wd], AF.Relu, [prk], [Rk])
                    mm(pa[:, 0:wd], d_[:, h, :], R[:, 0:wd], h == 0, h == 7, [dgk, Rk], [pak])
                cp('dve', sc[:, ks], pa[:, 0:wd], [pak], ['sc'])
            tt('dve', sc[:, Lk - 128:Lk], sc[:, Lk - 128:Lk], C('NEGq'), ALU.add, ['sc', 'cst'], ['sc'])
            if Lk > TOPK:
                S.op('dve', lambda e, Lk=Lk: e.tensor_reduce(lo, sc[:, 0:Lk - 128], AX.X, ALU.min), ['sc'], ['lo'])
                S.op('dve', lambda e, Lk=Lk: e.tensor_reduce(hi, sc[:, 0:Lk], AX.X, ALU.max), ['sc'], ['hi'])
                tt('dve', w0, hi, lo, ALU.subtract, ['hi', 'lo'], ['w0'])
                ts('dve', wtab, C('pow2'), w0, None, ALU.mult, None, ['cst', 'w0'], ['wtab'])
                for k in range(NIT):
                    tt('dve', mid, lo, wtab[:, k:k + 1], ALU.add, ['lo', 'wtab'], ['mid'])
                    memset('dve', cnt, 0.0, ['cnt'])
                    ts('dve', mrow[:, 0:Lk], sc[:, 0:Lk], mid, 0.0, ALU.is_ge, ALU.add, ['sc', 'mid'], ['mrow', 'cnt'], accum_out=cnt)
                    stt('dve', inc, cnt, TOPK - 0.5, wtab[:, k:k + 1], ALU.is_ge, ALU.mult, ['cnt', 'wtab'], ['inc'])
                    tt('dve', lo, lo, inc, ALU.add, ['lo', 'inc'], ['lo'])
            else:
                memset('dve', lo, -1.0e38, ['lo'])
            if L.debug and Lk > TOPK:
                dbt = L.A([128, 4])
                cp('dve', dbt[:, 0:1], lo, ['lo'], ['dbt'])
                cp('dve', dbt[:, 1:2], hi, ['hi'], ['dbt'])
                cp('dve', dbt[:, 2:3], cnt, ['cnt'], ['dbt'])
                cp('dve', dbt[:, 3:4], w0, ['w0'], ['dbt'])
                S.dma('sp', L.tk_s[128 * qt:128 * (qt + 1), :], dbt, ['dbt'], ['tk_s%d' % qt])
            ts('dve', mrow[:, 0:Lk], sc[:, 0:Lk], lo, None, ALU.is_ge, None, ['sc', 'lo'], ['mrow'])
            nkb = Lk // 128
            for b0 in range(0, nkb, 8):
                nb = min(8, nkb - b0)
                pt, ptk = psr.next()
                pv = pt.bitcast(BF16)
                for j in range(nb):
                    tr(pv[:, 128 * j:128 * (j + 1)], mrow[:, 128 * (b0 + j):128 * (b0 + j + 1)], ident_b, ['mrow', 'ident_b'], [ptk])
                ts('dve', maskT[:, b0:b0 + nb, 128 * i:128 * (i + 1)], pv[:, 0:128 * nb].rearrange("p (j t) -> p j t", j=nb),
                   30000.0, -30000.0, ALU.mult, ALU.add, [ptk], ['maskT'])
        nkb = 4 * Q + 4
        for h in range(8):
            g = h // 4
            hp = slice(64 * g, 64 * g + 64)
            pa, pak = pacc.next()
            for kb in range(nkb):
                pr, prk = psr.next()
                mm(pr[:, 0:512], kT2[hp, 128 * kb:128 * (kb + 1)], qT[hp, h % 4, :], True, False, ['kT2', 'qT'], [prk])
                mm(pr[:, 0:512], ident_b, maskT[:, kb, :], False, True, ['ident_b', 'maskT'], [prk])
                E, Ek = Er.next()
                act(E, pr[:, 0:512], AF.Exp, [prk], [Ek], scale=0.125)
                mm(pa[0:65, 0:512], Vt[:, kb, 65 * g:65 * g + 65], E, kb == 0, kb == nkb - 1, ['Vt', Ek], [pak])
            S.op('dve', lambda e, pa=pa: e.reciprocal(rden[64:65, :], pa[64:65, 0:512]), [pak], ['rden'])
            pr, prk = psr.next()
            mm(pr[0:64, 0:512], C('ones')[64:65, 0:64], rden[64:65, :], True, True, ['cst', 'rden'], [prk])
            cp('act', bcs, pr[0:64, 0:512], [prk], ['bcs'])
            o_, ok_ = Ob.next()
            tt('dve', o_, pa[0:64, 0:512], bcs, ALU.mult, [pak, 'bcs'], [ok_])
            S.dma('sp', L.catT_s[h // 2, 64 * (h % 2):64 * (h % 2) + 64, qs], o_, [ok_], ['catT_at%d_%d' % (Q, h)])


def phase_ffn(S, Ld):
    L = _NS(Ld)
    A, mm, tr, act, cp, tt, ts, stt, memset, bview = L.A, L.mm, L.tr, L.act, L.cp, L.tt, L.ts, L.stt, L.memset, L.bview
    PB, PBk, C, l, NT, SQ, NQB = L.PB, L.PBk, L.C, L.l, L.NT, L.SQ, L.NQB
    ident_b = L.ident_b
    L.new_phase()
    ppf = A([128, 32])
    S.dma('sp', ppf[:, 0:16], L.pp_d[l, :, 0:16], ['pp_d'], ['ppf'])
    gs = A([128, 2])
    o0 = PP['gssd'][0]
    S.dma('sp', gs, L.pp_d[l, :, o0:o0 + 2], ['pp_d'], ['gs'])
    Wout_b = A([128, 8, 1024], BF16)
    Wup_b = A([128, 8, 4096], BF16)
    stg = Rot([A([128, 2048]), A([128, 2048])], 'stgf')
    n_ = 0
    for c in range(8):
        for hf in range(1):
            s_, sk = stg.next()
            S.dma('sp' if n_ % 2 == 0 else 'pool', s_[:, 0:1024], L.wout_d[l, c * 128:(c + 1) * 128, :], ['wout_d'], [sk])
            n_ += 1
            if c >= 6:
                ts('dve', Wout_b[:, c, :], s_[:, 0:1024], gs[:, c - 6:c - 5], None, ALU.mult, None, [sk, 'gs'], ['Wout_b'])
            else:
                cp('dve', Wout_b[:, c, :], s_[:, 0:1024], [sk], ['Wout_b'])
    for c in range(8):
        for hf in range(2):
            s_, sk = stg.next()
            S.dma('sp' if n_ % 2 == 0 else 'pool', s_, L.wup_d[l, c * 128:(c + 1) * 128, hf * 2048:(hf + 1) * 2048], ['wup_d'], [sk])
            n_ += 1
            ts('dve' if hf == 0 else 'pool', Wup_b[:, c, hf * 2048:(hf + 1) * 2048], s_, ppf[:, 8 + c:9 + c], None, ALU.mult, None,
               [sk, 'ppf'], ['Wup_b'])
    xrot = Rot([A([128, 1024]), A([128, 1024])], 'fx')
    crot = Rot([A([128, 8, 128], BF16), A([128, 8, 128], BF16)], 'fc')
    x1r = Rot([A([128, 1024]), A([128, 1024])], 'x1')
    h2b = A([128, 1024], BF16)
    h2T = A([128, 8, 512], BF16)
    junk = A([128, 1024]); ss = A([128, 1])
    rl = Rot([A([128, 512]), A([128, 512])], 'rl')
    aTr = Rot([A([128, 4, 512], BF16), A([128, 4, 512], BF16)], 'aT')
    pup = Rot([PB[2], PB[3], PB[4], PB[5]], PBk[2:6])
    for Q in range(NQB):
        for j in range(4):
            t = 4 * Q + j
            tok = slice(t * 128, (t + 1) * 128)
            xt, xk = xrot.next()
            S.dma('sp', xt, L.xsrc[tok, :], ['xin%d' % t], [xk])
            ct, ck = crot.next()
            rk = ['catT_s5_%d' % t, 'catT_ssd_%d' % t] + ['catT_at%d_%d' % (Q, h) for h in range(8)]
            S.dma('pool', ct, L.catT_s[:, :, tok].rearrange("c p t -> p c t"), rk, [ck])
            for n in range(2):
                for c in range(8):
                    mm(PB[n][:, 0:512], ct[:, c, :], Wout_b[:, c, n * 512:(n + 1) * 512], c == 0, c == 7, [ck, 'Wout_b'], [PBk[n]])
            x1, x1k = x1r.next()
            for n in range(2):
                tt('dve', x1[:, n * 512:(n + 1) * 512], xt[:, n * 512:(n + 1) * 512], PB[n][:, 0:512], ALU.add, [xk, PBk[n]], [x1k])
            S.dma('sp', L.xb_d[tok, :], x1, [x1k], ['xin%d' % t])
            memset('pool', ss, 0.0, ['ss'])
            act(junk, x1, AF.Square, [x1k], ['junkF', 'ss'], accum_out=ss)
            L.rstd_inplace(ss, 'ss', 1.0 / 1024)
            ts('dve', h2b, x1, ss, None, ALU.mult, None, [x1k, 'ss'], ['h2b'])
            pT = bview(7)
            for c in range(8):
                tr(pT[:, c * 128:(c + 1) * 128], h2b[:, c * 128:(c + 1) * 128], ident_b, ['h2b', 'ident_b'], [PBk[7]])
            cp('act', h2T[:, :, j * 128:(j + 1) * 128], pT.rearrange("p (c t) -> p c t", c=8), [PBk[7]], ['h2T'])
        for f in range(32):
            pu, puk = pup.next()
            for c in range(8):
                mm(pu[:, 0:512], Wup_b[:, c, f * 128:(f + 1) * 128], h2T[:, c, :], c == 0, c == 7, ['Wup_b', 'h2T'], [puk])
            r_, rk_ = rl.next()
            act(r_, pu[:, 0:512], AF.Relu, [puk], [rk_])
            if f % 4 == 0:
                aT, aTk = aTr.next()
            tt('pool' if f % 2 == 0 else 'dve', aT[:, f % 4, :], r_, r_, ALU.mult, [rk_], [aTk])
            if f % 4 == 3:
                S.dma('sp' if (f // 4) % 2 == 0 else 'pool', L.aT_s[f - 3:f + 1, :, Q * 512:(Q + 1) * 512].rearrange("f p t -> p f t"), aT,
                      [aTk], ['aT_s%d_%d' % (Q, f // 4)])
    L.new_phase()
    Wdn_b = A([128, 32, 1024], BF16)
    stg = Rot([A([128, 2048]), A([128, 2048])], 'stgd')
    for f in range(0, 32, 2):
        s_, sk = stg.next()
        S.dma('sp' if (f // 2) % 2 == 0 else 'pool', s_.rearrange("p (a n) -> p a n", a=2),
              L.wdn_d[l, f * 128:(f + 2) * 128, :].rearrange("(a p) n -> p a n", p=128), ['wdn_d'], [sk])
        cp('dve' if (f // 2) % 2 == 0 else 'pool', Wdn_b[:, f:f + 2, :].rearrange("p a n -> p (a n)"), s_, [sk], ['Wdn_b'])
    aTl = Rot([A([128, 32, 512], BF16), A([128, 32, 512], BF16)], 'aTl')
    xrot = Rot([A([128, 1024]), A([128, 1024])], 'dx')
    orot = Rot([A([128, 1024]), A([128, 1024])], 'do')
    pd = Rot([PB[0], PB[1], PB[2], PB[3]], PBk[0:4])
    for Q in range(NQB):
        a_, ak = aTl.next()
        for f4 in range(8):
            S.dma('sp' if f4 % 2 == 0 else 'pool', a_[:, 4 * f4:4 * f4 + 4, :],
                  L.aT_s[4 * f4:4 * f4 + 4, :, Q * 512:(Q + 1) * 512].rearrange("f p t -> p f t"), ['aT_s%d_%d' % (Q, f4)], [ak])
        for j in range(4):
            t = 4 * Q + j
            tok = slice(t * 128, (t + 1) * 128)
            xt, xk = xrot.next()
            S.dma('sp', xt, L.xb_d[tok, :], ['xin%d' % t], [xk])
            ot, ok_ = orot.next()
            for n in range(2):
                p_, pk = pd.next()
                for f in range(32):
                    mm(p_[:, 0:512], a_[:, f, j * 128:(j + 1) * 128], Wdn_b[:, f, n * 512:(n + 1) * 512], f == 0, f == 31, [ak, 'Wdn_b'], [pk])
                tt('dve', ot[:, n * 512:(n + 1) * 512], xt[:, n * 512:(n + 1) * 512], p_[:, 0:512], ALU.add, [xk, pk], [ok_])
            S.dma('pool', L.xdst[tok, :], ot, [ok_], ['xin%d' % t])


def make_in_maps(inp, SQ, NL, nb):
    cst, cs = host_consts(SQ)
    common = {
        'w_in': np.ascontiguousarray(np.asarray(inp['w_in'], np.float32)[:NL][:, :, W_IN_PERM]),
        'w_out': np.ascontiguousarray(np.asarray(inp['w_out'], np.float32)[:NL]),
        'w_up': np.ascontiguousarray(np.asarray(inp['w_up'], np.float32)[:NL]),
        'w_down': np.ascontiguousarray(np.asarray(inp['w_down'], np.float32)[:NL]),
        'glu_w': np.ascontiguousarray(np.asarray(inp['s5_glu_w'], np.float32)[:NL]),
        'pp': np.stack([host_pp(inp, l) for l in range(NL)]),
        'cst': cst, 'cs': cs,
    }
    return [dict(common, x=np.ascontiguousarray(np.asarray(inp['x'][b], np.float32))) for b in range(nb)]


_NC_CACHE = {}
FUSED = True


def _layer_slice(inp, l):
    out = {}
    for k, v in inp.items():
        out[k] = v if k == 'x' else np.asarray(v)[l:l + 1]
    return out


def kernel(**inputs):
    inp = {k: np.asarray(v) for k, v in inputs.items()}
    B, SQ, _ = inp['x'].shape
    NL = inp['w_in'].shape[0]
    if FUSED:
        key = (SQ, NL)
        if key not in _NC_CACHE:
            _NC_CACHE[key] = build(SQ, NL)
        in_maps = make_in_maps(inp, SQ, NL, B)
        res = run_bass_kernel_spmd(_NC_CACHE[key], in_maps, core_ids=list(range(B)))
        return np.stack([np.asarray(r['out'], np.float32) for r in res.results]).astype(np.float32)
    key = (SQ, 1)
    if key not in _NC_CACHE:
        _NC_CACHE[key] = build(SQ, 1)
    x = np.asarray(inp['x'], np.float32)
    for l in range(NL):
        li = _layer_slice(inp, l)
        li['x'] = x
        in_maps = make_in_maps(li, SQ, 1, B)
        res = run_bass_kernel_spmd(_NC_CACHE[key], in_maps, core_ids=list(range(B)))
        x = np.stack([np.asarray(r['out'], np.float32) for r in res.results]).astype(np.float32)
    return x
```
